# Optimizing a Trainium2 kernel written in Bass

```python
import math
import jax, jax.numpy as jnp
from jax import lax
import numpy as np

D_MODEL = 1024
BATCH = 8
SEQ = 4096
DEPTH = 1

MIX_WIDTH = D_MODEL
HEAD_DIM = 64
N_HEADS = (MIX_WIDTH // 2) // HEAD_DIM
ATTN_WIDTH = N_HEADS * HEAD_DIM
SSM_GROUP_CH = 16
SSM_STATE = 64
SSM_WIDTH = MIX_WIDTH - ATTN_WIDTH
SSM_GROUPS = SSM_WIDTH // SSM_GROUP_CH
DILATED_BRANCHES = ((128, 1), (512, 4), (2048, 16))
PLE_DIM = 256
N_EXPERTS = 64
TOP_K = 8
N_EXPERT_GROUPS = 8
TOPK_GROUPS = 4
EXPERT_HIDDEN = 256
SHARED_HIDDEN = 256
ROUTED_SCALE = 2.5
BLOCK_ROWS = 128
LN_EPS = 1e-5

kernel_name = 'hybrid_dilated_attn_s5_moe_deepnorm'


def layer_norm(x, g, b):
    xf = x.astype(jnp.float32)
    mu = xf.mean(-1, keepdims=True)
    var = jnp.square(xf - mu).mean(-1, keepdims=True)
    y = (xf - mu) * lax.rsqrt(var + LN_EPS) * g.astype(jnp.float32) + b.astype(jnp.float32)
    return y.astype(x.dtype)


def alibi_slopes(n_heads):
    return jnp.exp2(-8.0 * jnp.arange(1, n_heads + 1, dtype=jnp.float32) / n_heads)


def dilated_branch(q, k, v, dil, n_keys, slopes):
    b, s, h, e = q.shape
    span = dil * n_keys
    sp = -(-s // span) * span
    L = sp // dil
    nb = L // n_keys
    pad = ((0, 0), (0, sp - s), (0, 0), (0, 0))

    def split(t):
        t = jnp.pad(t, pad).reshape(b, L, dil, h, e).transpose(0, 2, 1, 3, 4)
        return t.reshape(b, dil, nb, n_keys, h, e)

    def with_prev(t):
        prev = jnp.pad(t, ((0, 0), (0, 0), (1, 0), (0, 0), (0, 0), (0, 0)))[:, :, :-1]
        return jnp.concatenate([prev, t], axis=3)

    qb = split(q)
    kw = with_prev(split(k))
    vw = with_prev(split(v))
    scores = jnp.einsum('brnqhe,brnkhe->brnhqk', qb, kw,
                        preferred_element_type=jnp.float32) * (HEAD_DIM ** -0.5)
    qi = jnp.arange(n_keys)[:, None]
    kj = jnp.arange(2 * n_keys)[None, :]
    steps = qi + n_keys - kj
    in_band = (steps >= 0) & (steps <= n_keys)
    first_blk = (jnp.arange(nb) == 0)[:, None, None]
    valid = in_band[None] & ~(first_blk & (kj < n_keys)[None])
    bias = -slopes[:, None, None] * (steps * dil).astype(jnp.float32)[None]
    scores = jnp.where(valid[None, None, :, None], scores + bias, -jnp.inf)
    m = scores.max(-1, keepdims=True)
    pexp = jnp.exp(scores - m)
    l = pexp.sum(-1)
    o = jnp.einsum('brnhqk,brnkhe->brnqhe', pexp, vw.astype(jnp.float32))
    o = o / jnp.swapaxes(l, 3, 4)[..., None]
    lse = jnp.swapaxes(m[..., 0] + jnp.log(l), 3, 4)
    o = o.reshape(b, dil, L, h, e).transpose(0, 2, 1, 3, 4).reshape(b, sp, h, e)[:, :s]
    lse = lse.reshape(b, dil, L, h).transpose(0, 2, 1, 3).reshape(b, sp, h)[:, :s]
    return o, lse


def dilated_attention(q, k, v):
    slopes = alibi_slopes(q.shape[2])
    outs, lses = [], []
    for window, dil in DILATED_BRANCHES:
        o, lse = dilated_branch(q, k, v, dil, window // dil, slopes)
        outs.append(o)
        lses.append(lse)
    w = jax.nn.softmax(jnp.stack(lses, 0), axis=0)
    return jnp.sum(w[..., None] * jnp.stack(outs, 0), axis=0)


def s5_mixer(u, lam_re, lam_im, log_dt, b_re, b_im, c_re, c_im, d_skip, w_glu, b_glu):
    b, s, w = u.shape
    f32 = jnp.float32
    uf = u.astype(f32).reshape(b, s, SSM_GROUPS, SSM_GROUP_CH)
    lam = lax.complex(lam_re.astype(f32), lam_im.astype(f32))
    dt = jnp.exp(log_dt.astype(f32))[:, None]
    lam_bar = jnp.exp(lam * dt)
    gain = (lam_bar - 1.0) / lam
    b_bar = gain[..., None] * lax.complex(b_re.astype(f32), b_im.astype(f32))
    bu = jnp.einsum('bsgc,gnc->bsgn', uf.astype(jnp.complex64), b_bar)
    a = jnp.broadcast_to(lam_bar, bu.shape)

    def combine(left, right):
        a1, x1 = left
        a2, x2 = right
        return a1 * a2, a2 * x1 + x2

    _, states = lax.associative_scan(combine, (a, bu), axis=1)
    y = (jnp.einsum('gcn,bsgn->bsgc', c_re.astype(f32), states.real)
         - jnp.einsum('gcn,bsgn->bsgc', c_im.astype(f32), states.imag))
    y = (y + d_skip.astype(f32) * uf).reshape(b, s, w)
    y = jax.nn.gelu(y)
    y = y * jax.nn.sigmoid(y @ w_glu.astype(f32) + b_glu.astype(f32))
    return y.astype(u.dtype)


def routed_moe(h, w_router, router_bias, w_gate, w_up, w_down):
    t, d = h.shape
    scores = jax.nn.sigmoid((h @ w_router).astype(jnp.float32))
    sel = scores + router_bias.astype(jnp.float32)
    per_group = N_EXPERTS // N_EXPERT_GROUPS
    grp_score = lax.top_k(sel.reshape(t, N_EXPERT_GROUPS, per_group), 2)[0].sum(-1)
    _, top_g = lax.top_k(grp_score, TOPK_GROUPS)
    gmask = jax.nn.one_hot(top_g, N_EXPERT_GROUPS).sum(1) > 0
    emask = jnp.repeat(gmask, per_group, axis=1)
    _, idx = lax.top_k(jnp.where(emask, sel, -jnp.inf), TOP_K)
    wts = jnp.take_along_axis(scores, idx, axis=1)
    wts = wts / wts.sum(-1, keepdims=True) * ROUTED_SCALE

    n_assign = t * TOP_K
    flat_e = idx.reshape(n_assign)
    flat_w = wts.reshape(n_assign)
    flat_tok = jnp.arange(n_assign, dtype=jnp.int32) // TOP_K
    order = jnp.argsort(flat_e)
    se = flat_e[order]
    counts = jnp.bincount(flat_e, length=N_EXPERTS)
    start = jnp.cumsum(counts) - counts
    pcounts = (counts + BLOCK_ROWS - 1) // BLOCK_ROWS * BLOCK_ROWS
    pend = jnp.cumsum(pcounts)
    pstart = pend - pcounts
    dest = pstart[se] + (jnp.arange(n_assign) - start[se])
    n_blocks = -(-n_assign // BLOCK_ROWS) + N_EXPERTS
    n_rows = n_blocks * BLOCK_ROWS
    buf_tok = jnp.full((n_rows,), t, jnp.int32).at[dest].set(flat_tok[order])
    buf_w = jnp.zeros((n_rows,), jnp.float32).at[dest].set(flat_w[order])
    block_e = jnp.minimum(jnp.searchsorted(pend, jnp.arange(n_blocks) * BLOCK_ROWS, side='right'),
                          N_EXPERTS - 1)
    h_pad = jnp.concatenate([h, jnp.zeros((1, d), h.dtype)], axis=0)

    def run_block(args):
        tok, e, wb = args
        xb = h_pad[tok]
        hid = jax.nn.silu(xb @ w_gate[e]) * (xb @ w_up[e])
        return (hid @ w_down[e]) * wb[:, None].astype(h.dtype)

    yb = lax.map(run_block, (buf_tok.reshape(n_blocks, BLOCK_ROWS), block_e,
                             buf_w.reshape(n_blocks, BLOCK_ROWS)))
    y = jnp.zeros((t + 1, d), h.dtype).at[buf_tok].add(yb.reshape(n_rows, d))
    return y[:t]


def setup_inputs(seed: int = 0) -> dict:
    key = jax.random.key(seed)
    ks = jax.random.split(key, 32)
    f32 = jnp.float32
    beta = (8.0 * DEPTH) ** -0.25
    L, D, G, N, C = DEPTH, D_MODEL, SSM_GROUPS, SSM_STATE, SSM_GROUP_CH
    E, H, HS = N_EXPERTS, EXPERT_HIDDEN, SHARED_HIDDEN
    nrm = lambda k, shp, sc: jax.random.normal(k, shp, f32) * sc
    in_cols = 3 * ATTN_WIDTH + SSM_WIDTH
    lam_im0 = jnp.pi * jnp.arange(N, dtype=f32)
    return {
        'x': nrm(ks[0], (BATCH, SEQ, D), 1.0),
        'p': nrm(ks[1], (DEPTH, BATCH, SEQ, PLE_DIM), 1.0),
        'w_in': nrm(ks[2], (L, D, in_cols), D ** -0.5),
        'lam_re': -0.5 + nrm(ks[3], (L, G, N), 0.01),
        'lam_im': lam_im0 + nrm(ks[4], (L, G, N), 0.01),
        'log_dt': jax.random.uniform(ks[5], (L, G), f32, math.log(1e-3), math.log(1e-1)),
        'b_re': nrm(ks[6], (L, G, N, C), (2.0 * C) ** -0.5),
        'b_im': nrm(ks[7], (L, G, N, C), (2.0 * C) ** -0.5),
        'c_re': nrm(ks[8], (L, G, C, N), (2.0 * N) ** -0.5),
        'c_im': nrm(ks[9], (L, G, C, N), (2.0 * N) ** -0.5),
        'd_skip': nrm(ks[10], (L, G, C), 1.0),
        'w_glu': nrm(ks[11], (L, SSM_WIDTH, SSM_WIDTH), SSM_WIDTH ** -0.5),
        'b_glu': nrm(ks[12], (L, SSM_WIDTH), 0.01),
        'w_out': nrm(ks[13], (L, MIX_WIDTH, D), beta * MIX_WIDTH ** -0.5),
        'ln1_g': 1.0 + nrm(ks[14], (L, D), 0.02),
        'ln1_b': nrm(ks[15], (L, D), 0.02),
        'w_router': nrm(ks[16], (L, D, E), D ** -0.5),
        'router_bias': nrm(ks[17], (L, E), 0.01),
        'w_gate': nrm(ks[18], (L, E, D, H), D ** -0.5),
        'w_up': nrm(ks[19], (L, E, D, H), D ** -0.5),
        'w_down': nrm(ks[20], (L, E, H, D), beta * H ** -0.5),
        'ws_gate': nrm(ks[21], (L, D, HS), D ** -0.5),
        'ws_up': nrm(ks[22], (L, D, HS), D ** -0.5),
        'ws_down': nrm(ks[23], (L, HS, D), beta * HS ** -0.5),
        'w_ple': nrm(ks[24], (L, PLE_DIM, D), beta * PLE_DIM ** -0.5),
        'w_ple_gate': nrm(ks[25], (L, D, D), D ** -0.5),
        'ln2_g': 1.0 + nrm(ks[26], (L, D), 0.02),
        'ln2_b': nrm(ks[27], (L, D), 0.02),
    }


def reference(x, p, w_in, lam_re, lam_im, log_dt, b_re, b_im, c_re, c_im, d_skip,
              w_glu, b_glu, w_out, ln1_g, ln1_b, w_router, router_bias, w_gate, w_up,
              w_down, ws_gate, ws_up, ws_down, w_ple, w_ple_gate, ln2_g, ln2_b):
    alpha = (2.0 * DEPTH) ** 0.25
    b, s, d = x.shape
    h = x
    for i in range(DEPTH):
        z = h @ w_in[i]
        q = z[..., :ATTN_WIDTH].reshape(b, s, N_HEADS, HEAD_DIM)
        k = z[..., ATTN_WIDTH:2 * ATTN_WIDTH].reshape(b, s, N_HEADS, HEAD_DIM)
        v = z[..., 2 * ATTN_WIDTH:3 * ATTN_WIDTH].reshape(b, s, N_HEADS, HEAD_DIM)
        u = z[..., 3 * ATTN_WIDTH:]
        attn = dilated_attention(q, k, v).reshape(b, s, ATTN_WIDTH).astype(h.dtype)
        ssm = s5_mixer(u, lam_re[i], lam_im[i], log_dt[i], b_re[i], b_im[i], c_re[i],
                       c_im[i], d_skip[i], w_glu[i], b_glu[i])
        mix = jnp.concatenate([attn, ssm], axis=-1) @ w_out[i]
        h = layer_norm(alpha * h + mix, ln1_g[i], ln1_b[i])
        hf = h.reshape(b * s, d)
        routed = routed_moe(hf, w_router[i], router_bias[i], w_gate[i], w_up[i], w_down[i])
        shared = (jax.nn.silu(hf @ ws_gate[i]) * (hf @ ws_up[i])) @ ws_down[i]
        ffn = (routed + shared).reshape(b, s, d)
        ple = (p[i] @ w_ple[i]) * jax.nn.sigmoid(h @ w_ple_gate[i])
        h = layer_norm(alpha * h + ffn + ple, ln2_g[i], ln2_b[i])
    return h
```

```python
import math
from contextlib import ExitStack
import numpy as np
import ml_dtypes
import concourse.bass as bass
import concourse.mybir as mybir
from concourse.bass_utils import run_bass_kernel_spmd

F32 = mybir.dt.float32
BF16 = mybir.dt.bfloat16
I32 = mybir.dt.int32
AF = mybir.ActivationFunctionType
ALU = mybir.AluOpType
AX = mybir.AxisListType

S = 4096
D = 1024
ALPHA = 2.0 ** 0.25
EPS = 1e-5
TWO_PI_S = 2.0 * math.pi * (1.0 - 2e-6)
NQ = 8
QT = S // NQ
NTT = QT // 128
NTB = QT // 512
ENGS = ['pe', 'act', 'dve', 'pool', 'sp']


class TK:
    def __init__(self):
        self.w = None
        self.r = {}


class Tl:
    def __init__(self, t, tk=None):
        self.t = t
        self.tk = tk or TK()

    def __getitem__(self, idx):
        return self.t[idx]


class Prog:
    def __init__(self, nc, es):
        self.nc = nc
        self.es = es
        self.ops = {e: [] for e in ENGS}
        self.cnt = {e: 0 for e in ENGS}
        self.seen = {e: {} for e in ENGS}
        self.sem = {}
        for e in ENGS:
            self.sem[e] = es.enter_context(nc.semaphore("s_" + e))
        self.out_streams = []

    def stream(self, name):
        self.sem[name] = self.es.enter_context(self.nc.semaphore("d_" + name))
        self.cnt[name] = 0
        return name

    def _waits(self, eng, reads, writes):
        deps = {}

        def add(d):
            if d is None:
                return
            f, c = d
            if c > deps.get(f, 0):
                deps[f] = c
        for t in reads:
            add(t.tk.w)
        for t in writes:
            add(t.tk.w)
            for f, c in t.tk.r.items():
                add((f, c))
        waits = []
        for f, c in deps.items():
            if f == eng and eng == 'pe':
                continue
            if c > self.seen[eng].get(f, 0):
                waits.append((f, c))
                self.seen[eng][f] = c
        return waits

    def op(self, eng, fn, reads=(), writes=()):
        waits = self._waits(eng, reads, writes)
        self.cnt[eng] += 1
        n = self.cnt[eng]
        self.ops[eng].append((waits, fn, (eng, 1)))
        for t in reads:
            t.tk.r[eng] = n
        for t in writes:
            t.tk.w = (eng, n)
            t.tk.r = {}

    def dma(self, q, st, fn, reads=(), writes=()):
        waits = self._waits(q, reads, writes)
        self.cnt[st] += 16
        n = self.cnt[st]
        self.ops[q].append((waits, fn, (st, 16)))
        for t in reads:
            t.tk.r[st] = n
        for t in writes:
            t.tk.w = (st, n)
            t.tk.r = {}

    def barrier(self):
        snap = dict(self.cnt)
        for e in ENGS:
            waits = []
            for f, c in snap.items():
                if f != e and c > self.seen[e].get(f, 0):
                    waits.append((f, c))
                    self.seen[e][f] = c
            self.ops[e].append((waits, None, None))

    def emit(self, eobj, eng):
        for waits, fn, inc in self.ops[eng]:
            for f, c in waits:
                eobj.wait_ge(self.sem[f], c)
            if fn is not None:
                ins = fn(eobj)
                ins.then_inc(self.sem[inc[0]], inc[1])


def bc_last(ap, n):
    sh = list(ap.shape)
    return ap.unsqueeze(len(sh)).broadcast_to(sh + [n])


def build(debug=None):
    debug = debug or {}
    nc = bass.Bass("TRN2", target_bir_lowering=False)
    es = ExitStack()
    P = Prog(nc, es)

    def din(name, shape, dt=F32):
        return nc.dram_tensor(name, list(shape), dt, kind="ExternalInput").ap()

    xT_d = din("xT", [D, S]) if not debug.get("ffn_only") else None
    x_d = din("x", [S, D])
    pT_d = din("pT", [256, S])
    w_in_d = din("w_in", [D, 2048]) if not debug.get("ffn_only") else None
    w_out_d = din("w_out", [D, D])
    ln1g_d = din("ln1_g", [D]); ln1b_d = din("ln1_b", [D])
    ln2g_d = din("ln2_g", [D]); ln2b_d = din("ln2_b", [D])
    wr_d = din("w_router", [D, 64]); rb_d = din("router_bias", [64])
    SPARSE = debug.get("sparse", True)
    if SPARSE:
        wg2_d = din("wg2", [8192, 2048]); wu2_d = din("wu2", [8192, 2048]); wd2_d = din("wd2", [8192, 2048])
        c_tri_d = din("c_tri", [128, 128]); c_tokid_d = din("c_tokid", [128, 32]); c_pcol_d = din("c_pcol", [128, 1])
        c_bvals_d = din("c_bvals", [128, 320]); c_prefill_d = din("c_prefill", [128, 1280])
        Hs_d = nc.dram_tensor("Hs", [S + 1, D], BF16, kind="Internal").ap()
        Zs_d = nc.dram_tensor("Zs", [S, D], F32, kind="Internal").ap()
        btw_d = nc.dram_tensor("btw", [40960, 4], F32, kind="Internal").ap()
        yrows_d = nc.dram_tensor("yrows", [40960, D], BF16, kind="Internal").ap()
        wbf_d = nc.dram_tensor("wbf", [8192, 6144], BF16, kind="Internal").ap()
        wg_d = wu_d = wd_d = None
    else:
        wg_d = din("w_gate", [64, D, 256]); wu_d = din("w_up", [64, D, 256]); wd_d = din("w_down", [64, 256, D])
    wsg_d = din("ws_gate", [D, 256]); wsu_d = din("ws_up", [D, 256]); wsd_d = din("ws_down", [256, D])
    wple_d = din("w_ple", [256, D]); wpg_d = din("w_ple_gate", [D, D])
    ident_d = din("c_ident", [128, 128])
    out_d = nc.dram_tensor("out", [S, D], F32, kind="ExternalOutput").ap()
    mixT_dbg = din("mixT_dbg", [D, S]) if debug.get("ffn_only") else None
    if not debug.get("ffn_only"):
        lamre_d = din("lamre2", [128, 32]); lamim_d = din("lamim2", [128, 32]); logdt_d = din("logdt2", [128, 32])
        bre_d = din("bre2", [128, 32, 16]); bim_d = din("bim2", [128, 32, 16])
        cre_d = din("cre2", [128, 32, 16]); cim_d = din("cim2", [128, 32, 16]); dsk_d = din("dsk2", [128, 32])
        wglu_d = din("w_glu", [512, 512]); bglu_d = din("bglu2", [128, 4])
        c_selF_d = din("c_selF2", [128, 4096]); c_selU_d = din("c_selU2", [128, 2048]); c_maskK_d = din("c_maskK", [128, 128])
        c_kvals_d = din("c_kvals", [128, 33]); c_half_d = din("c_half", [128, 4]); c_maskb_d = din("c_maskb", [128, 256])
        c_qaug_d = din("c_qaug", [8, 4, S]); c_kaug_d = din("c_kaug", [4, S])
        c_lsel_d = din("c_lsel", [65, 128]); c_place_d = din("c_place", [65, 2, 128])
        c_perm_d = din("c_perm", [128, 128])
    mixout_d = (nc.dram_tensor("mixout", [D, S], F32, kind="ExternalOutput").ap() if debug.get("mix_only") else None)

    def sb(name, shape, dt=F32):
        return Tl(es.enter_context(nc.sbuf_tensor(name, list(shape), dt)))

    def ps(name):
        return Tl(es.enter_context(nc.psum_tensor(name, [128, 512], F32)))

    PS = [ps("ps%d" % i) for i in range(8)]

    ident = sb("ident", [128, 128], F32)
    identb = sb("identb", [128, 128], BF16)
    st_c = P.stream("const")
    P.dma('sp', st_c, lambda e: e.dma_start(out=ident[:], in_=ident_d[:, :]), writes=[ident])
    P.op('dve', lambda e: e.tensor_copy(out=identb[:], in_=ident[:]), reads=[ident], writes=[identb])

    IOA = bass.IndirectOffsetOnAxis
    if SPARSE:
        st_hs = [P.stream("hs0"), P.stream("hs1")]
        st_zs = [P.stream("zs0"), P.stream("zs1")]
        mskb = sb("mskb", [128, NTT, 64], BF16)
        cum = sb("cum", [128, 64], F32)
        trib = sb("trib", [128, 128], BF16); onesb = sb("onesb", [128, 128], BF16)
        P.dma('pool', P.stream("tri"), lambda e: e.dma_start(out=trib[:], in_=c_tri_d[:, :]), writes=[trib])
        P.op('dve', lambda e: e.memset(onesb[:], 1.0), writes=[onesb])
        P.op('dve', lambda e: e.memset(cum[:], 0.0), writes=[cum])

    es_mix = ExitStack()
    mixT = Tl(es_mix.enter_context(nc.sbuf_tensor("mixT", [128, 8, debug.get("s_alloc", S)], BF16)))

    _regs = {}

    def breg(e, val):
        if val not in _regs:
            _regs[val] = e.to_reg(val)
        return _regs[val]

    def flush():
        P.barrier()
        _regs.clear()
        with nc.Block() as block:
            @block.tensor
            def _(e):
                P.emit(e, 'pe')

            @block.scalar
            def _(e):
                P.emit(e, 'act')

            @block.vector
            def _(e):
                P.emit(e, 'dve')

            @block.gpsimd
            def _(e):
                P.emit(e, 'pool')

            @block.sync
            def _(e):
                P.emit(e, 'sp')
        for e_ in ENGS:
            P.ops[e_] = []

    _psk = [0]

    def nextps():
        _psk[0] += 1
        return PS[_psk[0] % 8]

    def V(fn, reads, writes):
        P.op('dve', fn, reads=reads, writes=writes)

    def A(fn, reads, writes):
        P.op('act', fn, reads=reads, writes=writes)

    def evac(k, out_ap, pb, out_tile, in_ap=None):
        src = pb[:, :] if in_ap is None else in_ap
        if k % 2 == 0:
            A(lambda e: e.activation(out=out_ap, in_=src, func=AF.Copy), [pb], [out_tile])
        else:
            V(lambda e: e.tensor_copy(out=out_ap, in_=src), [pb], [out_tile])

    mix_lo = Tl(mixT.t)
    mix_hi = Tl(mixT.t)
    wv_in = w_in_d.rearrange("(c p) n -> p c n", p=128) if w_in_d is not None else None
    xv = xT_d.rearrange("(c p) t -> p c t", p=128) if xT_d is not None else None

    def load_xT(stack):
        xT_ = Tl(stack.enter_context(nc.sbuf_tensor("xT_%d" % len(P.sem), [128, 8, S], BF16)))
        st_ = P.stream("xT%d" % len(P.sem))
        for c in range(8):
            P.dma('pool', st_, lambda e, c=c: e.dma_start(out=xT_[:, c, :], in_=xv[:, c, :]), writes=[xT_])
        return xT_

    if debug.get("ffn_only"):
        st_m = P.stream("mixdbg")
        mv = mixT_dbg.rearrange("(c p) t -> p c t", p=128)
        for c in range(8):
            P.dma('pool', st_m, lambda e, c=c: e.dma_start(out=mixT[:, c, :], in_=mv[:, c, 0:debug.get('s_alloc', S)]), writes=[mixT])
    else:
        do_ssm = debug.get("do_ssm", True)
        do_att = debug.get("do_att", True)
        if SPARSE:
            st_wst_in = P.stream("wst_in"); st_wst_out = P.stream("wst_out")
        if do_ssm:
            es1 = ExitStack()
            xT = load_xT(es1)
            winu = Tl(es1.enter_context(nc.sbuf_tensor("winu", [128, 8, 512], BF16)))
            P.dma('pool', P.stream("winu"), lambda e: e.dma_start(out=winu[:], in_=wv_in[:, :, 1536:2048]), writes=[winu])
            k = 0
            for gb in range(4):
                for tb in range(8):
                    pb = PS[k % 2]
                    for kc in range(8):
                        P.op('pe', lambda e, pb=pb, kc=kc, gb=gb, tb=tb: e.matmul(
                            pb[:, :], winu[:, kc, gb * 128:(gb + 1) * 128], xT[:, kc, tb * 512:(tb + 1) * 512],
                            start=(kc == 0), stop=(kc == 7)), reads=[winu, xT], writes=[pb])
                    evac(k, mixT[:, 4 + gb, tb * 512:(tb + 1) * 512], pb, mix_hi)
                    k += 1
            flush()
            es1.close()

            es2 = ExitStack()

            def sb2(name, shape, dt=F32):
                return Tl(es2.enter_context(nc.sbuf_tensor(name, list(shape), dt)))

            def ld(name, shape, src, dt=F32, q='sp'):
                t = sb2(name, shape, dt)
                P.dma(q, P.stream("l_" + name), lambda e: e.dma_start(out=t[:], in_=src), writes=[t])
                return t
            lamre = ld("lamre", [128, 32], lamre_d[:, :]); lamim = ld("lamim", [128, 32], lamim_d[:, :])
            logdt = ld("logdt", [128, 32], logdt_d[:, :])
            bre = ld("bre", [128, 32, 16], bre_d[:, :, :]); bim = ld("bim", [128, 32, 16], bim_d[:, :, :])
            cre = ld("cre", [128, 32, 16], cre_d[:, :, :]); cim = ld("cim", [128, 32, 16], cim_d[:, :, :])
            dsk = ld("dsk", [128, 32], dsk_d[:, :]); kvals = ld("kvals", [128, 33], c_kvals_d[:, :])
            halfc = ld("halfc", [128, 4], c_half_d[:, :]); maskK = ld("maskK", [128, 128], c_maskK_d[:, :])
            perm = ld("perm", [128, 128], c_perm_d[:, :])
            selF = ld("selF", [128, 4, 8, 128], c_selF_d.rearrange("p (a j m) -> p a j m", a=4, j=8), BF16, 'pool')
            selU = ld("selU", [128, 4, 8, 64], c_selU_d.rearrange("p (a j m) -> p a j m", a=4, j=8), BF16, 'pool')
            wglu = ld("wglu", [128, 4, 512], wglu_d.rearrange("(c p) n -> p c n", p=128), BF16, 'pool')
            bglu = ld("bglu", [128, 4], bglu_d[:, :])
            X1 = sb2("X1", [128, 32, 8]); X2 = sb2("X2", [128, 32, 8])
            Y1 = sb2("Y1", [128, 32, 16]); Y2 = sb2("Y2", [128, 32, 16])
            crt = sb2("crt", [128, 32, 9]); cst = sb2("cst", [128, 32, 9]); csn = sb2("csn", [128, 32, 9])

            es3 = ExitStack()

            def sb3(name, shape, dt=F32):
                return Tl(es3.enter_context(nc.sbuf_tensor(name, list(shape), dt)))
            dt_t = sb3("dt_t", [128, 32]); a_t = sb3("a_t", [128, 32]); u1 = sb3("u1", [128, 32]); s32 = sb3("s32", [128, 32])
            i32 = sb3("i32", [128, 32], I32)
            T1 = sb3("T1", [128, 32, 33]); T2 = sb3("T2", [128, 32, 33]); T3 = sb3("T3", [128, 32, 33]); T4 = sb3("T4", [128, 32, 33])
            TI = sb3("TI", [128, 32, 33], I32)
            Gre = sb3("Gre", [128, 32, 8]); Gim = sb3("Gim", [128, 32, 8]); G8 = sb3("G8", [128, 32, 8])
            gre = sb3("gre", [128, 32]); gim = sb3("gim", [128, 32]); nre = sb3("nre", [128, 32]); den = sb3("den", [128, 32])
            Y16 = sb3("Y16", [128, 32, 16])

            def rr(x, xi, tmp):
                V(lambda e: e.tensor_copy(out=xi[:], in_=x[:]), [x], [xi])
                V(lambda e: e.tensor_copy(out=tmp[:], in_=xi[:]), [xi], [tmp])
                V(lambda e: e.tensor_tensor(out=x[:], in0=x[:], in1=tmp[:], op=ALU.subtract), [x, tmp], [x])
                V(lambda e: e.tensor_scalar(out=tmp[:], in0=x[:], scalar1=0.5, scalar2=None, op0=ALU.is_gt), [x], [tmp])
                V(lambda e: e.tensor_tensor(out=x[:], in0=x[:], in1=tmp[:], op=ALU.subtract), [x, tmp], [x])
                V(lambda e: e.tensor_scalar(out=tmp[:], in0=x[:], scalar1=-0.5, scalar2=None, op0=ALU.is_lt), [x], [tmp])
                V(lambda e: e.tensor_tensor(out=x[:], in0=x[:], in1=tmp[:], op=ALU.add), [x, tmp], [x])

            A(lambda e: e.activation(out=dt_t[:], in_=logdt[:], func=AF.Exp), [logdt], [dt_t])
            V(lambda e: e.tensor_tensor(out=a_t[:], in0=lamre[:], in1=dt_t[:], op=ALU.mult), [lamre, dt_t], [a_t])
            V(lambda e: e.scalar_tensor_tensor(out=u1[:], in0=lamim[:], scalar=1.0 / (2.0 * math.pi), in1=dt_t[:],
                                               op0=ALU.mult, op1=ALU.mult), [lamim, dt_t], [u1])
            rr(u1, i32, s32)
            kv_b = kvals.t[:, :].unsqueeze(1).broadcast_to([128, 32, 33])
            V(lambda e: e.tensor_tensor(out=T1[:], in0=bc_last(u1[:, :], 33), in1=kv_b, op=ALU.mult), [u1, kvals], [T1])
            rr(T1, TI, T2)
            A(lambda e: e.activation(out=T3[:], in_=T1[:], func=AF.Sin, scale=TWO_PI_S), [T1], [T3])
            V(lambda e: e.tensor_scalar(out=T1[:], in0=T1[:], scalar1=0.25, scalar2=None, op0=ALU.add), [T1], [T1])
            V(lambda e: e.tensor_scalar(out=T2[:], in0=T1[:], scalar1=0.5, scalar2=None, op0=ALU.is_gt), [T1], [T2])
            V(lambda e: e.tensor_tensor(out=T1[:], in0=T1[:], in1=T2[:], op=ALU.subtract), [T1, T2], [T1])
            A(lambda e: e.activation(out=T2[:], in_=T1[:], func=AF.Sin, scale=TWO_PI_S), [T1], [T2])
            V(lambda e: e.tensor_tensor(out=T4[:], in0=bc_last(a_t[:, :], 33), in1=kv_b, op=ALU.mult), [a_t, kvals], [T4])
            A(lambda e: e.activation(out=T4[:], in_=T4[:], func=AF.Exp), [T4], [T4])
            V(lambda e: e.tensor_tensor(out=T2[:], in0=T2[:], in1=T4[:], op=ALU.mult), [T2, T4], [T2])
            V(lambda e: e.tensor_tensor(out=T3[:], in0=T3[:], in1=T4[:], op=ALU.mult), [T3, T4], [T3])
            Lre, Lim = T2, T3
            V(lambda e: e.tensor_scalar(out=nre[:], in0=Lre[:, :, 16], scalar1=-1.0, scalar2=None, op0=ALU.add), [Lre], [nre])
            V(lambda e: e.tensor_tensor(out=den[:], in0=lamre[:], in1=lamre[:], op=ALU.mult), [lamre], [den])
            V(lambda e: e.tensor_tensor(out=s32[:], in0=lamim[:], in1=lamim[:], op=ALU.mult), [lamim], [s32])
            V(lambda e: e.tensor_tensor(out=den[:], in0=den[:], in1=s32[:], op=ALU.add), [den, s32], [den])
            V(lambda e: e.reciprocal(out=den[:], in_=den[:]), [den], [den])
            V(lambda e: e.tensor_tensor(out=gre[:], in0=nre[:], in1=lamre[:], op=ALU.mult), [nre, lamre], [gre])
            V(lambda e: e.tensor_tensor(out=s32[:], in0=Lim[:, :, 16], in1=lamim[:], op=ALU.mult), [Lim, lamim], [s32])
            V(lambda e: e.tensor_tensor(out=gre[:], in0=gre[:], in1=s32[:], op=ALU.add), [gre, s32], [gre])
            V(lambda e: e.tensor_tensor(out=gre[:], in0=gre[:], in1=den[:], op=ALU.mult), [gre, den], [gre])
            V(lambda e: e.tensor_tensor(out=gim[:], in0=Lim[:, :, 16], in1=lamre[:], op=ALU.mult), [Lim, lamre], [gim])
            V(lambda e: e.tensor_tensor(out=s32[:], in0=nre[:], in1=lamim[:], op=ALU.mult), [nre, lamim], [s32])
            V(lambda e: e.tensor_tensor(out=gim[:], in0=gim[:], in1=s32[:], op=ALU.subtract), [gim, s32], [gim])
            V(lambda e: e.tensor_tensor(out=gim[:], in0=gim[:], in1=den[:], op=ALU.mult), [gim, den], [gim])
            V(lambda e: e.tensor_tensor(out=Gre[:], in0=Lre[:, :, 0:8], in1=bc_last(gre[:, :], 8), op=ALU.mult), [Lre, gre], [Gre])
            V(lambda e: e.tensor_tensor(out=G8[:], in0=Lim[:, :, 0:8], in1=bc_last(gim[:, :], 8), op=ALU.mult), [Lim, gim], [G8])
            V(lambda e: e.tensor_tensor(out=Gre[:], in0=Gre[:], in1=G8[:], op=ALU.subtract), [Gre, G8], [Gre])
            V(lambda e: e.tensor_tensor(out=Gim[:], in0=Lre[:, :, 0:8], in1=bc_last(gim[:, :], 8), op=ALU.mult), [Lre, gim], [Gim])
            V(lambda e: e.tensor_tensor(out=G8[:], in0=Lim[:, :, 0:8], in1=bc_last(gre[:, :], 8), op=ALU.mult), [Lim, gre], [G8])
            V(lambda e: e.tensor_tensor(out=Gim[:], in0=Gim[:], in1=G8[:], op=ALU.add), [Gim, G8], [Gim])
            h0c, h1c, sgc, nh1c = halfc.t[:, 0:1], halfc.t[:, 1:2], halfc.t[:, 2:3], halfc.t[:, 3:4]

            def comb(out, p_, pc, q_, qc, op1, tmp):
                V(lambda e: e.tensor_scalar(out=tmp[:], in0=q_, scalar1=qc, scalar2=None, op0=ALU.mult), [Gre, Gim, T2, T3, halfc], [tmp])
                V(lambda e: e.scalar_tensor_tensor(out=out[:], in0=p_, scalar=pc, in1=tmp[:], op0=ALU.mult, op1=op1),
                  [Gre, Gim, T2, T3, halfc, tmp], [out])
            comb(X1, Gre[:], h0c, Gim[:], h1c, ALU.add, G8)
            comb(X2, Gre[:], h1c, Gim[:], h0c, ALU.subtract, G8)
            comb(Y1, Lre[:, :, 8:24], h0c, Lim[:, :, 8:24], h1c, ALU.subtract, Y16)
            comb(Y2, Lre[:, :, 8:24], nh1c, Lim[:, :, 8:24], h0c, ALU.subtract, Y16)
            V(lambda e: e.tensor_copy(out=crt[:], in_=Lre[:, :, 24:33]), [Lre], [crt])
            V(lambda e: e.tensor_scalar(out=cst[:], in0=Lim[:, :, 24:33], scalar1=sgc, scalar2=None, op0=ALU.mult), [Lim, halfc], [cst])
            V(lambda e: e.tensor_scalar(out=csn[:], in0=cst[:], scalar1=-1.0, scalar2=None, op0=ALU.mult), [cst], [csn])
            flush()
            es3.close()

            AT = sb2("AT", [128, 8, 128]); BM = sb2("BM", [128, 8, 128]); M2f = sb2("M2f", [128, 8, 128])
            tmp4 = sb2("tmp4", [128, 8, 128])
            M1b = sb2("M1b", [128, 8, 128], BF16)
            M2b = sb2("M2b", [128, 8, 128], BF16); Kmb = sb2("Kmb", [128, 8, 128], BF16)
            tmpK = sb2("tmpK", [128, 4, 128])
            U = sb2("U", [128, 8, 512], BF16)
            SA = sb2("SA", [128, 4, 512]); SB = sb2("SB", [128, 4, 512])
            Sp = [sb2("Sp%d" % i, [128, 4, 512], BF16) for i in range(2)]
            Yg = sb2("Yg", [128, 8, 512], BF16)
            for i in range(2):
                V(lambda e, i=i: e.memset(Sp[i][:, :, 0:1], 0.0), [], [Sp[i]])

            def v4(t):
                return t.t[:].rearrange("p g (j c) -> p g j c", j=8)

            def outer(dst, xa, ya, xb, yb, g0):
                def bx(x):
                    return x.t[:, g0:g0 + 8, :].unsqueeze(3).broadcast_to([128, 8, 8, 16])

                def by(y):
                    return y.t[:, g0:g0 + 8, :].unsqueeze(2).broadcast_to([128, 8, 8, 16])
                V(lambda e: e.tensor_tensor(out=v4(dst), in0=bx(xa), in1=by(ya), op=ALU.mult), [xa, ya], [dst])
                V(lambda e: e.tensor_tensor(out=v4(tmp4), in0=bx(xb), in1=by(yb), op=ALU.mult), [xb, yb], [tmp4])
                V(lambda e: e.tensor_tensor(out=dst[:], in0=dst[:], in1=tmp4[:], op=ALU.add), [dst, tmp4], [dst])

            Y1B = Tl(Y1.t[:, :, 0:8], Y1.tk); Y2B = Tl(Y2.t[:, :, 0:8], Y2.tk)
            Y1C = Tl(Y1.t[:, :, 8:16], Y1.tk); Y2C = Tl(Y2.t[:, :, 8:16], Y2.tk)
            ek = 0
            if SPARSE:
                wst = sb2("wst", [128, 4096], BF16)
            SAg = [Tl(SA.t[:, gl, :]) for gl in range(4)]
            SBg = [Tl(SB.t[:, gl, :]) for gl in range(4)]
            for gb in range(4):
                g0 = gb * 8
                if SPARSE:
                    for e16 in range(8):
                        ee = gb * 8 + e16
                        rows = slice(ee * 128, (ee + 1) * 128)
                        for a_, wd_ in enumerate((wg2_d, wu2_d)):
                            P.dma('pool', st_wst_in, lambda e, rows=rows, a_=a_, wd_=wd_: e.dma_start(
                                out=wst[:, a_ * 2048:(a_ + 1) * 2048], in_=wd_[rows, :]), writes=[wst])
                        P.dma('sp', st_wst_out, lambda e, rows=rows: e.dma_start(out=wbf_d[rows, 0:4096], in_=wst[:, 0:4096]), reads=[wst])
                        P.dma('pool', st_wst_in, lambda e, rows=rows: e.dma_start(out=wst[:, 0:2048], in_=wd2_d[rows, :]), writes=[wst])
                        P.dma('sp', st_wst_out, lambda e, rows=rows: e.dma_start(out=wbf_d[rows, 4096:6144], in_=wst[:, 0:2048]), reads=[wst])
                outer(AT, X1, bre, X2, bim, g0)
                outer(BM, Y1B, cre, Y2B, cim, g0)
                outer(M2f, Y1C, cre, Y2C, cim, g0)
                V(lambda e: e.tensor_copy(out=M2b[:], in_=M2f[:]), [M2f], [M2b])
                for hf in range(2):
                    pk = nextps()
                    for gl in range(4):
                        g = hf * 4 + gl
                        P.op('pe', lambda e, pk=pk, gl=gl, g=g: e.matmul(pk[:, gl * 128:(gl + 1) * 128], AT[:, g, :], BM[:, g, :],
                                                                        start=True, stop=True), reads=[AT, BM], writes=[pk])
                    V(lambda e, pk=pk: e.tensor_tensor(out=tmpK[:], in0=pk[:, :].rearrange("p (g m) -> p g m", g=4),
                                                       in1=maskK.t[:, :].unsqueeze(1).broadcast_to([128, 4, 128]), op=ALU.mult),
                      [pk, maskK], [tmpK])
                    for gl in range(4):
                        g = hf * 4 + gl
                        V(lambda e, gl=gl, g=g, g0=g0: e.scalar_tensor_tensor(
                            out=Kmb[:, g, :], in0=ident[:], scalar=dsk[:, g0 + g:g0 + g + 1], in1=tmpK[:, gl, :],
                            op0=ALU.mult, op1=ALU.add), [ident, dsk, tmpK], [Kmb])
                for src, dstb in ((AT, M1b),):
                    for hf in range(2):
                        pk = nextps()
                        for gl in range(4):
                            g = hf * 4 + gl
                            P.op('pe', lambda e, pk=pk, gl=gl, g=g, src=src: e.transpose(pk[:, gl * 128:(gl + 1) * 128], src[:, g, :], ident[:]),
                                 reads=[src, ident], writes=[pk])
                        evac(ek, dstb[:, hf * 4:(hf + 1) * 4, :], pk, dstb, pk[:, :].rearrange("p (g m) -> p g m", g=4))
                        ek += 1
                for g in range(8):
                    a_, par = g // 4, g % 4
                    pk = nextps()
                    for j in range(8):
                        P.op('pe', lambda e, pk=pk, a_=a_, par=par, j=j, gb=gb: e.matmul(
                            pk[:, :], selF[64 * a_:64 * a_ + 64, par, j, :], mixT[64 * a_:64 * a_ + 64, 4 + gb, j:S:8],
                            start=(j == 0), stop=(j == 7)), reads=[selF, mix_hi], writes=[pk])
                    evac(ek, U[:, g, :], pk, U)
                    ek += 1
                for un in range(2):
                    for gl in range(4):
                        g = un * 4 + gl
                        pk = nextps()
                        P.op('pe', lambda e, pk=pk, g=g: e.matmul(pk[:, :], M1b[:, g, :], U[:, g, :], start=True, stop=True),
                             reads=[M1b, U], writes=[pk])
                        evac(ek, SAg[gl][:, :], pk, SAg[gl])
                        ek += 1
                    spt = Sp[un]
                    srcg, dstg = SAg, SBg
                    for s_ in range(9):
                        d = 1 << s_
                        for gl in range(4):
                            G = g0 + un * 4 + gl
                            src0, dst0 = srcg[gl], dstg[gl]
                            A(lambda e, src0=src0, dst0=dst0, d=d: e.activation(out=dst0[:, 0:d], in_=src0[:, 0:d], func=AF.Copy),
                              [src0], [dst0])
                            pk = nextps()
                            P.op('pe', lambda e, pk=pk, src0=src0: e.matmul(pk[:, :], perm[:, :], src0[:, :], start=True, stop=True),
                                 reads=[perm, src0], writes=[pk])
                            V(lambda e, G=G, d=d, s_=s_, src0=src0, dst0=dst0: e.scalar_tensor_tensor(
                                out=dst0[:, d:512], in0=src0[:, 0:512 - d], scalar=crt[:, G, s_:s_ + 1],
                                in1=src0[:, d:512], op0=ALU.mult, op1=ALU.add), [src0, crt], [dst0])
                            V(lambda e, G=G, d=d, s_=s_, pk=pk, dst0=dst0: e.scalar_tensor_tensor(
                                out=dst0[:, d:512], in0=pk[:, 0:512 - d], scalar=cst[:, G, s_:s_ + 1],
                                in1=dst0[:, d:512], op0=ALU.mult, op1=ALU.add), [pk, cst, dst0], [dst0])
                        srcg, dstg = dstg, srcg
                    for gl in range(4):
                        Sfin = srcg[gl]
                        A(lambda e, Sfin=Sfin, spt=spt, gl=gl: e.activation(out=spt[:, gl, 1:512], in_=Sfin[:, 0:511], func=AF.Copy), [Sfin], [spt])
                    for gl in range(4):
                        g = un * 4 + gl
                        spt = Sp[un]
                        pk = nextps()
                        P.op('pe', lambda e, pk=pk, g=g: e.matmul(pk[:, :], Kmb[:, g, :], U[:, g, :], start=True, stop=False),
                             reads=[Kmb, U], writes=[pk])
                        P.op('pe', lambda e, pk=pk, g=g, gl=gl, spt=spt: e.matmul(pk[:, :], M2b[:, g, :], spt[:, gl, :], start=False, stop=True),
                             reads=[M2b, spt], writes=[pk])
                        A(lambda e, pk=pk, g=g: e.activation(out=Yg[:, g, :], in_=pk[:, :], func=AF.Gelu_apprx_tanh), [pk], [Yg])
                for i in range(8):
                    pk = nextps()
                    for g in range(8):
                        a_, par = g // 4, g % 4
                        P.op('pe', lambda e, pk=pk, a_=a_, par=par, i=i, g=g: e.matmul(
                            pk[64 * a_:64 * a_ + 64, :], selU[:, par, i, :], Yg[:, g, :], start=(par == 0), stop=(par == 3)),
                            reads=[selU, Yg], writes=[pk])
                    evac(ek, mixT[:, gb, i:S:8], pk, mix_lo)
                    ek += 1
            sgl = [sb2("sgl%d" % i, [128, 512], BF16) for i in range(2)]
            k = 0
            for mo in range(4):
                for tb in range(8):
                    tsl = slice(tb * 512, (tb + 1) * 512)
                    pk = nextps()
                    for ki in range(4):
                        P.op('pe', lambda e, pk=pk, ki=ki, mo=mo, tsl=tsl: e.matmul(
                            pk[:, :], wglu[:, ki, mo * 128:(mo + 1) * 128], mixT[:, ki, tsl], start=(ki == 0), stop=(ki == 3)),
                            reads=[wglu, mix_lo], writes=[pk])
                    sg_ = sgl[k % 2]
                    A(lambda e, pk=pk, sg_=sg_, mo=mo: e.activation(out=sg_[:], in_=pk[:, :], func=AF.Sigmoid, bias=bglu[:, mo:mo + 1], scale=1.0),
                      [pk, bglu], [sg_])
                    V(lambda e, sg_=sg_, mo=mo, tsl=tsl: e.tensor_tensor(out=mixT[:, 4 + mo, tsl], in0=sg_[:], in1=mixT[:, mo, tsl], op=ALU.mult),
                      [sg_, mix_lo], [mix_hi])
                    k += 1
            flush()
            es2.close()

        if do_att:
            es4 = ExitStack()

            def sb4(name, shape, dt=F32):
                return Tl(es4.enter_context(nc.sbuf_tensor(name, list(shape), dt)))
            xT = load_xT(es4)
            wq = sb4("wq", [128, 8, 64], BF16); wk = sb4("wk", [128, 8, 64], BF16); wvv = sb4("wvv", [128, 8, 128], BF16)
            st_w = [P.stream("wq"), P.stream("wk"), P.stream("wv")]
            qT = sb4("qT", [68, S], BF16); kT = sb4("kT", [68, S], BF16)
            st_qa = P.stream("qaug")
            vt = [sb4("vt%d" % b_, [128, 32, 2, 65], BF16) for b_ in range(3)]
            Oa = sb4("Oa", [65, S], F32)
            NPT = 3
            PT = [sb4("PT%d" % i, [128, 256], BF16) for i in range(NPT)]
            mask01 = sb4("mask01", [128, 256], BF16)
            P.dma('pool', P.stream("maskb"), lambda e: e.dma_start(out=mask01[:], in_=c_maskb_d[:, :]), writes=[mask01])
            P.dma('pool', P.stream("kaug"), lambda e: e.dma_start(out=kT[64:68, :], in_=c_kaug_d[:, :]), writes=[kT])
            lsel = sb4("lsel", [65, 128]); place = sb4("place", [65, 2, 128])
            P.dma('sp', P.stream("lsel"), lambda e: e.dma_start(out=lsel[:], in_=c_lsel_d[:, :]), writes=[lsel])
            P.dma('sp', P.stream("place"), lambda e: e.dma_start(out=place[:], in_=c_place_d[:, :, :]), writes=[place])
            Rt = sb4("Rt", [128, 512])
            if SPARSE:
                wst4 = sb4("wst4", [128, 2048], BF16)
            for b_ in range(3):
                V(lambda e, b_=b_: e.memset(vt[b_][:, :, :, 64:65], 1.0), [], [vt[b_]])
            units = []
            for n in range(32):
                units.append((0, 128 * n, 1, n - 1 if n > 0 else None, n))
            for r in range(4):
                for n in range(8):
                    units.append((1, 512 * n + r, 4, (r * 8 + n - 1) if n > 0 else None, r * 8 + n))
            for r in range(16):
                for n in range(2):
                    units.append((2, 2048 * n + r, 16, (r * 2 + n - 1) if n > 0 else None, r * 2 + n))

            def tok_ap(t, rows, base, dil):
                return t.t[rows, base:base + dil * 127 + 1:dil] if dil > 1 else t.t[rows, base:base + 128]
            PSS = [PS[0], PS[1]]; PSO = [PS[2], PS[3]]; PSP = [PS[4], PS[5]]; PSV = [PS[6], PS[7]]
            r68 = slice(0, 68)
            for h in range(8):
                pbase = (h % 2) * 64
                for i_, (wt_, off) in enumerate(((wq, 0), (wk, 512))):
                    P.dma('pool', st_w[i_], lambda e, wt_=wt_, off=off, h=h: e.dma_start(
                        out=wt_[:], in_=wv_in[:, :, off + h * 64:off + (h + 1) * 64]), writes=[wt_])
                if h % 2 == 0:
                    P.dma('pool', st_w[2], lambda e, h=h: e.dma_start(out=wvv[:], in_=wv_in[:, :, 1024 + h * 64:1024 + (h + 2) * 64]),
                          writes=[wvv])
                P.dma('pool', st_qa, lambda e, h=h: e.dma_start(out=qT[64:68, :], in_=c_qaug_d[h]), writes=[qT])
                if SPARSE:
                    for e4 in range(4):
                        ee = 32 + h * 4 + e4
                        rows = slice(ee * 128, (ee + 1) * 128)
                        for a_, wd_ in enumerate((wg2_d, wu2_d, wd2_d)):
                            P.dma('pool', st_wst_in, lambda e, rows=rows, wd_=wd_: e.dma_start(out=wst4[:, :], in_=wd_[rows, :]), writes=[wst4])
                            P.dma('sp', st_wst_out, lambda e, rows=rows, a_=a_: e.dma_start(out=wbf_d[rows, a_ * 2048:(a_ + 1) * 2048], in_=wst4[:, :]),
                                  reads=[wst4])
                k = 0
                for wt_, dst, scl in ((wq, qT, 0.125), (wk, kT, 1.0)):
                    for tb in range(8):
                        pk = PSP[k % 2]
                        for kc in range(8):
                            P.op('pe', lambda e, pk=pk, kc=kc, tb=tb, wt_=wt_: e.matmul(
                                pk[0:64, :], wt_[:, kc, :], xT[:, kc, tb * 512:(tb + 1) * 512], start=(kc == 0), stop=(kc == 7)),
                                reads=[wt_, xT], writes=[pk])
                        A(lambda e, pk=pk, dst=dst, tb=tb, scl=scl: e.activation(
                            out=dst[0:64, tb * 512:(tb + 1) * 512], in_=pk[0:64, :], func=AF.Copy, scale=scl), [pk], [dst])
                        k += 1
                k = 0
                for b_, dil in (((0, 1), (1, 4), (2, 16)) if h % 2 == 0 else ()):
                    for t4 in range(8):
                        pk = PSV[k % 2]
                        for tl in range(4):
                            tile_id = t4 * 4 + tl
                            if b_ == 0:
                                base = 128 * tile_id
                            elif b_ == 1:
                                base = 512 * (tile_id % 8) + tile_id // 8
                            else:
                                base = 2048 * (tile_id % 2) + tile_id // 2
                            for kc in range(8):
                                P.op('pe', lambda e, pk=pk, tl=tl, kc=kc, base=base, dil=dil: e.matmul(
                                    pk[:, tl * 128:(tl + 1) * 128],
                                    (xT[:, kc, base:base + dil * 127 + 1:dil] if dil > 1 else xT[:, kc, base:base + 128]),
                                    wvv[:, kc, :], start=(kc == 0), stop=(kc == 7)), reads=[xT, wvv], writes=[pk])
                        V(lambda e, pk=pk, b_=b_, t4=t4: e.tensor_copy(out=vt[b_][:, t4 * 4:(t4 + 1) * 4, :, 0:64],
                                                                       in_=pk[:, :].rearrange("p (t a e) -> p t a e", t=4, a=2)),
                          [pk], [vt[b_]])
                        k += 1

                def scores(ui):
                    b_, base, dil, tprev, tsame = units[ui]
                    pss = PSS[ui % 2]
                    qa = tok_ap(qT, r68, base, dil)
                    lo = 0 if tprev is not None else 128
                    P.op('pe', lambda e: e.matmul(pss[:, lo:256], identb[:, :], mask01[:, lo:256], start=True, stop=False),
                         reads=[identb, mask01], writes=[pss])
                    if tprev is not None:
                        kp = tok_ap(kT, r68, base - dil * 128, dil)
                        P.op('pe', lambda e: e.matmul(pss[:, 0:128], kp, qa, start=False, stop=False), reads=[kT, qT], writes=[pss])
                    ks = tok_ap(kT, r68, base, dil)
                    P.op('pe', lambda e: e.matmul(pss[:, 128:256], ks, qa, start=False, stop=True), reads=[kT, qT], writes=[pss])
                    pt = PT[ui % NPT]
                    A(lambda e: e.activation(out=pt[:, lo:256], in_=pss[:, lo:256], func=AF.Exp), [pss], [pt])

                def pv(ui):
                    b_, base, dil, tprev, tsame = units[ui]
                    hp = h % 2
                    pt = PT[ui % NPT]
                    pso = PSO[ui % 2]
                    if tprev is not None:
                        P.op('pe', lambda e: e.matmul(pso[0:65, 0:128], vt[b_][:, tprev, hp, :], pt[:, 0:128], start=True, stop=False),
                             reads=[vt[b_], pt], writes=[pso])
                    P.op('pe', lambda e: e.matmul(pso[0:65, 0:128], vt[b_][:, tsame, hp, :], pt[:, 128:256],
                                                  start=(tprev is None), stop=True), reads=[vt[b_], pt], writes=[pso])
                    oa = tok_ap(Oa, slice(0, 65), base, dil)
                    if b_ == 0:
                        V(lambda e: e.tensor_copy(out=oa, in_=pso[0:65, 0:128]), [pso], [Oa])
                    else:
                        V(lambda e: e.tensor_tensor(out=oa, in0=oa, in1=pso[0:65, 0:128], op=ALU.add), [pso, Oa], [Oa])
                nu = len(units)
                for ui in range(nu):
                    scores(ui)
                    if ui > 0:
                        pv(ui - 1)
                pv(nu - 1)
                for tb in range(8):
                    tsl = slice(tb * 512, (tb + 1) * 512)
                    pl_, po_ = PSP[0], PSP[1]
                    P.op('pe', lambda e, tsl=tsl, pl_=pl_: e.matmul(pl_[:, :], lsel[:, :], Oa[0:65, tsl], start=True, stop=True),
                         reads=[lsel, Oa], writes=[pl_])
                    P.op('pe', lambda e, tsl=tsl, po_=po_, h=h: e.matmul(po_[:, :], place[:, h % 2, :], Oa[0:65, tsl], start=True, stop=True),
                         reads=[place, Oa], writes=[po_])
                    V(lambda e, pl_=pl_: e.reciprocal(out=Rt[:], in_=pl_[:, :]), [pl_], [Rt])
                    V(lambda e, po_=po_, tsl=tsl, h=h, pbase=pbase: e.tensor_tensor(
                        out=mixT[pbase:pbase + 64, h // 2, tsl], in0=po_[pbase:pbase + 64, :], in1=Rt[pbase:pbase + 64, :], op=ALU.mult),
                        [po_, Rt], [mix_lo])
            flush()
            es4.close()

        if debug.get("mix_only"):
            st_md = P.stream("mixout")
            mo_v = mixout_d.rearrange("(c p) t -> p c t", p=128)
            for c in range(0 if do_att else 4, 8 if do_ssm else 4):
                P.dma('pool', st_md, lambda e, c=c: e.dma_start(out=mo_v[:, c, :], in_=mixT[:, c, :]), reads=[mixT, mix_lo, mix_hi])
            P.ops['pool'].append(([(st_md, P.cnt[st_md])], None, None))
            flush()
            es_mix.close()
            es.close()
            return nc

    esP = ExitStack()
    if SPARSE:
        wt_all = Tl(esP.enter_context(nc.sbuf_tensor("wt_all", [128, 32, 64], F32)))
        rank_all = Tl(esP.enter_context(nc.sbuf_tensor("rank_all", [128, 32, 64], F32)))
    esB = ExitStack()
    esA = ExitStack()
    sb_main = sb

    def sb(name, shape, dt=F32):
        return Tl(esA.enter_context(nc.sbuf_tensor(name, list(shape), dt)))

    acc = sb("acc", [128, 8, QT], F32); hT = sb("hT", [128, 8, QT], BF16)
    pTb = sb("pTb", [128, 2, QT], BF16); wtT = sb("wtT", [64, QT], BF16)
    acc_v = acc.t[:]; hT_v = hT.t[:]; pT_v = pTb.t[:]; wtT_v = wtT.t[:]

    slot = [sb("wslot%d" % i, [128, 6144], BF16) for i in range(2)] if not SPARSE else [None, None]
    st_slot = [P.stream("slot%d" % i) for i in range(2)]
    xt = [sb("xt%d" % i, [128, D], F32) for i in range(2)]
    st_x = [P.stream("x%d" % i) for i in range(2)]
    rt = [sb("rt%d" % i, [128, D], F32) for i in range(2)]
    lng = sb("lng", [128, D], F32); lnb = sb("lnb", [128, D], F32)
    st_ln = P.stream("ln"); st_ln2 = P.stream("ln2")
    wple = sb("wple", [128, 2, D], BF16)
    st_wple = P.stream("wple")
    wr = sb("wr", [128, 8, 64], F32)
    rbias = sb("rbias", [128, 64], F32)
    st_p = P.stream("pT")
    st_out = [P.stream("out%d" % i) for i in range(2)]
    sgt = [sb("sgt%d" % i, [128, 512], BF16) for i in range(2)]
    t1t = [sb("t1t%d" % i, [128, 512], BF16) for i in range(2)]
    actw = [[sb("actw%d_%d" % (i, j), [128, 512], BF16) for j in range(2)] for i in range(2)]
    oh = [sb("oh%d" % i, [64, 128], BF16) for i in range(2)] if not SPARSE else [None, None]
    sig = [sb("sig%d" % i, [128, 512], F32) for i in range(2)]
    stats = sb("stats", [128, 2, 6], F32)
    mv_t = sb("mv_t", [128, 2], F32)
    rstd = sb("rstd", [128, 1], F32)
    nb_t = sb("nb_t", [128, 1], F32)
    sc = sb("sc", [128, 64], F32); sel = sb("sel", [128, 64], F32); eq = sb("eq", [128, 64], F32)
    sel2 = sb("sel2", [128, 64], F32); m1 = sb("m1", [128, 8], F32); m2 = sb("m2", [128, 8], F32)
    gs = sb("gs", [128, 8], F32); top8 = sb("top8", [128, 8], F32); pen = sb("pen", [128, 8], F32)
    selm = sb("selm", [128, 64], F32); msk = sb("msk", [128, 64], F32); wsel = sb("wsel", [128, 64], F32)
    ssum = sb("ssum", [128, 1], F32); wt = sb("wt", [128, 64], F32)

    if not (debug.get('skipdma', 0) & 1):
      P.dma('sp', P.stream("wr"), lambda e: e.dma_start(out=wr[:], in_=wr_d.rearrange("(c p) e -> p c e", p=128)), writes=[wr])
    if not (debug.get('skipdma', 0) & 2):
      P.dma('sp', P.stream("rbias"), lambda e: e.dma_start(out=rbias[:], in_=rb_d.partition_broadcast(128)), writes=[rbias])
    if not (debug.get('skipdma', 0) & 4):
      P.dma('pool', st_wple, lambda e: e.dma_start(out=wple[:], in_=wple_d.rearrange("(c p) d -> p c d", p=128)),
          writes=[wple])

    def load_ln(g_d, b_d):
        P.dma('sp', st_ln, lambda e: e.dma_start(out=lng[:], in_=g_d.partition_broadcast(128)), writes=[lng])
        P.dma('sp', st_ln2, lambda e: e.dma_start(out=lnb[:], in_=b_d.partition_broadcast(128)), writes=[lnb])

    LN_POOL = [bool(debug.get('ln_pool', True))]

    def layer_norm(src, dst):
        for h in range(2):
            P.op('dve', lambda e, h=h: e.bn_stats(out=stats[:, h, :], in_=src[:, h * 512:(h + 1) * 512]),
                 reads=[src], writes=[stats])
        P.op('dve', lambda e: e.bn_aggr(out=mv_t[:], in_=stats[:].rearrange("p a b -> p (a b)")),
             reads=[stats], writes=[mv_t])
        P.op('act', lambda e: e.activation(out=rstd[:], in_=mv_t[:, 1:2], func=AF.Sqrt, bias=EPS, scale=1.0),
             reads=[mv_t], writes=[rstd])
        P.op('dve', lambda e: e.reciprocal(out=rstd[:], in_=rstd[:]), reads=[rstd], writes=[rstd])
        P.op('dve', lambda e: e.scalar_tensor_tensor(out=nb_t[:], in0=mv_t[:, 0:1], scalar=-1.0, in1=rstd[:],
                                                     op0=ALU.mult, op1=ALU.mult),
             reads=[mv_t, rstd], writes=[nb_t])
        P.op('dve', lambda e: e.tensor_scalar(out=dst[:], in0=src[:], scalar1=rstd[:, 0:1], scalar2=nb_t[:, 0:1],
                                              op0=ALU.mult, op1=ALU.add),
             reads=[src, nb_t, rstd], writes=[dst])
        eng_ = 'pool' if LN_POOL[0] else 'dve'
        P.op(eng_, lambda e: e.tensor_tensor(out=dst[:], in0=dst[:], in1=lng[:], op=ALU.mult),
             reads=[dst, lng], writes=[dst])
        P.op(eng_, lambda e: e.tensor_tensor(out=dst[:], in0=dst[:], in1=lnb[:], op=ALU.add),
             reads=[dst, lnb], writes=[dst])

    def load_big(w_d):
        wv = w_d.rearrange("(c p) d -> p c d", p=128)
        for h in range(2):
            sv = slot[h].t[:, 0:4096].rearrange("p (c d) -> p c d", c=8)
            P.dma('pool', st_slot[h], lambda e, h=h, sv=sv: e.dma_start(out=sv, in_=wv[:, :, h * 512:(h + 1) * 512]),
                  writes=[slot[h]])

    RES = SPARSE
    if RES:
        wout_t = sb("wout_t", [128, 8, D], BF16); wpg_t = sb("wpg_t", [128, 8, D], BF16); wsh_t = sb("wsh_t", [128, 6144], BF16)
        P.dma('pool', P.stream("wout_t"), lambda e: e.dma_start(out=wout_t[:], in_=w_out_d.rearrange("(c p) d -> p c d", p=128)), writes=[wout_t])
        P.dma('pool', P.stream("wpg_t"), lambda e: e.dma_start(out=wpg_t[:], in_=wpg_d.rearrange("(c p) d -> p c d", p=128)), writes=[wpg_t])
        st_wsh = P.stream("wsh_t")
        P.dma('pool', st_wsh, lambda e: e.dma_start(out=wsh_t.t[:, 0:2048].rearrange("p (c h) -> p c h", c=8),
                                                   in_=wsg_d.rearrange("(c p) h -> p c h", p=128)), writes=[wsh_t])
        P.dma('pool', st_wsh, lambda e: e.dma_start(out=wsh_t.t[:, 2048:4096].rearrange("p (c h) -> p c h", c=8),
                                                   in_=wsu_d.rearrange("(c p) h -> p c h", p=128)), writes=[wsh_t])
        P.dma('pool', st_wsh, lambda e: e.dma_start(out=wsh_t.t[:, 4096:6144].rearrange("p (c d) -> p c d", c=2),
                                                   in_=wsd_d.rearrange("(c p) d -> p c d", p=128)), writes=[wsh_t])

    def slot_big(h):
        return slot[h].t[:, 0:4096].rearrange("p (c d) -> p c d", c=8)

    def load_expert(e_idx, s):
        if e_idx < 64:
            g_d, u_d, d_d = wg_d[e_idx], wu_d[e_idx], wd_d[e_idx]
        else:
            g_d, u_d, d_d = wsg_d, wsu_d, wsd_d
        sg_v = slot[s].t[:, 0:2048].rearrange("p (c h) -> p c h", c=8)
        su_v = slot[s].t[:, 2048:4096].rearrange("p (c h) -> p c h", c=8)
        sd_v = slot[s].t[:, 4096:6144].rearrange("p (c d) -> p c d", c=2)
        P.dma('pool', st_slot[s], lambda e: e.dma_start(out=sg_v, in_=g_d.rearrange("(c p) h -> p c h", p=128)),
              writes=[slot[s]])
        P.dma('pool', st_slot[s], lambda e: e.dma_start(out=su_v, in_=u_d.rearrange("(c p) h -> p c h", p=128)),
              writes=[slot[s]])
        P.dma('pool', st_slot[s], lambda e: e.dma_start(out=sd_v, in_=d_d.rearrange("(c p) d -> p c d", p=128)),
              writes=[slot[s]])
        return sg_v, su_v, sd_v

    def sparse_moe():
        nonlocal sb, slot, xt, rt, lng, lnb, stats, mv_t, rstd, nb_t
        flush()
        esA.close()

        def sb(name, shape, dt=F32):
            return Tl(esB.enter_context(nc.sbuf_tensor(name, list(shape), dt)))
        slot = [sb("wslotB%d" % i, [128, 6144], BF16) for i in range(2)]
        xtb_ = sb("xtB0", [128, D], F32)
        xt = [xtb_, xtb_]
        rt = [sb("rtB%d" % i, [128, D], F32) for i in range(2)]
        lng = sb("lngB", [128, D], F32); lnb = sb("lnbB", [128, D], F32)
        stats = sb("statsB", [128, 2, 6], F32); mv_t = sb("mv_tB", [128, 2], F32)
        rstd = sb("rstdB", [128, 1], F32); nb_t = sb("nb_tB", [128, 1], F32)
        tokid = sb("tokid", [128, 32])
        zrow = sb("zrow", [128, 8], BF16)
        V(lambda e: e.memset(zrow[:], 0.0), [], [zrow])
        P.dma('sp', P.stream("zrow"), lambda e: e.dma_start(out=Hs_d[S:S + 1, :].rearrange("a (p c) -> (a p) c", p=128), in_=zrow[:]), reads=[zrow])
        pre = sb("pre", [128, 1280], F32)
        P.dma('sp', P.stream("pre"), lambda e: e.dma_start(out=pre[:], in_=c_prefill_d[:, :]), writes=[pre])
        btw_t = Tl(None)
        P.dma('sp', P.stream("prefill"), lambda e: e.dma_start(out=btw_d.rearrange("(p r) c -> p (r c)", p=128), in_=pre[:]),
              reads=[pre], writes=[btw_t]); pcol = sb("pcol", [128, 1]); bvals = sb("bvals", [128, 320])
        P.dma('sp', P.stream("tokid"), lambda e: e.dma_start(out=tokid[:], in_=c_tokid_d[:, :]), writes=[tokid])
        P.dma('sp', P.stream("pcol"), lambda e: e.dma_start(out=pcol[:], in_=c_pcol_d[:, :]), writes=[pcol])
        P.dma('sp', P.stream("bvals"), lambda e: e.dma_start(out=bvals[:], in_=c_bvals_d[:, :]), writes=[bvals])
        pc = sb("pc", [128, 64]); pci = sb("pci", [128, 64], I32); t64 = sb("t64", [128, 64]); t64b = sb("t64b", [128, 64])
        pend = sb("pend", [128, 64]); pstart = sb("pstart", [128, 64]); ones64 = sb("ones64", [128, 64])
        V(lambda e: e.memset(ones64[:], 1.0), [], [ones64])
        V(lambda e: e.tensor_scalar(out=t64[:], in0=cum[:], scalar1=127.0, scalar2=1.0 / 128.0, op0=ALU.add, op1=ALU.mult), [cum], [t64])
        V(lambda e: e.tensor_copy(out=pci[:], in_=t64[:]), [t64], [pci])
        V(lambda e: e.tensor_copy(out=pc[:], in_=pci[:]), [pci], [pc])
        V(lambda e: e.tensor_scalar(out=t64[:], in0=cum[:], scalar1=127.0, scalar2=None, op0=ALU.add), [cum], [t64])
        V(lambda e: e.tensor_scalar(out=t64b[:], in0=pc[:], scalar1=128.0, scalar2=None, op0=ALU.mult), [pc], [t64b])
        V(lambda e: e.tensor_tensor(out=t64b[:], in0=t64b[:], in1=t64[:], op=ALU.is_gt), [t64b, t64], [t64b])
        V(lambda e: e.tensor_tensor(out=pc[:], in0=pc[:], in1=t64b[:], op=ALU.subtract), [pc, t64b], [pc])
        V(lambda e: e.tensor_scalar(out=pc[:], in0=pc[:], scalar1=128.0, scalar2=None, op0=ALU.mult), [pc], [pc])
        V(lambda e: e.tensor_tensor_scan(out=pend[:], data0=ones64[:], data1=pc[:], initial=0.0, op0=ALU.mult, op1=ALU.add),
          [ones64, pc], [pend])
        V(lambda e: e.tensor_tensor(out=pstart[:], in0=pend[:], in1=pc[:], op=ALU.subtract), [pend, pc], [pstart])
        key = sb("key", [128, 64]); junk = sb("junk", [128, 64])
        d8 = sb("d8", [128, 32, 8]); d8i = sb("d8i", [128, 32, 8], I32); pay = sb("pay", [128, 32, 8, 4])
        V(lambda e: e.memset(pay[:], 0.0), [], [pay])
        for T_ in range(32):
            V(lambda e, T_=T_: e.tensor_tensor(out=key[:], in0=rank_all[:, T_, :], in1=pstart[:], op=ALU.add), [rank_all, pstart], [key])
            V(lambda e, T_=T_: e.tensor_scalar(out=junk[:], in0=wt_all[:, T_, :], scalar1=0.0, scalar2=None, op0=ALU.is_gt), [wt_all], [junk])
            V(lambda e, T_=T_: e.scalar_tensor_tensor(out=key[:], in0=key[:], scalar=1.0, in1=junk[:], op0=ALU.add, op1=ALU.mult),
              [key, junk], [key])
            V(lambda e: e.tensor_scalar(out=key[:], in0=key[:], scalar1=-1.0, scalar2=None, op0=ALU.add), [key], [key])
            V(lambda e, T_=T_: e.max(out=d8[:, T_, :], in_=key[:]), [key], [d8])
            for k in range(8):
                V(lambda e, T_=T_, k=k: e.scalar_tensor_tensor(out=junk[:], in0=key[:], scalar=d8[:, T_, k:k + 1], in1=wt_all[:, T_, :],
                                                              op0=ALU.is_equal, op1=ALU.mult, accum_out=pay[:, T_, k, 1:2]),
                  [key, d8, wt_all], [junk, pay])
            V(lambda e, T_=T_: e.tensor_copy(out=pay[:, T_, :, 0], in_=tokid[:, T_:T_ + 1].broadcast_to([128, 8])), [tokid], [pay])
        V(lambda e: e.tensor_copy(out=d8i[:], in_=d8[:]), [d8], [d8i])
        st_sc = P.stream("scatter")
        for T_ in range(32):
            for k in range(8):
                P.dma('pool', st_sc, lambda e, T_=T_, k=k: e.indirect_dma_start(
                    out=btw_d[:, :], out_offset=IOA(ap=d8i[:, T_, k:k + 1], axis=0), in_=pay[:, T_, k, :], in_offset=None,
                    bounds_check=breg(e, 40959), oob_is_err=False), reads=[d8i, pay, btw_t])
        blk = sb("blk", [128, 320]); cmpb = sb("cmpb", [128, 16, 64]); idxw = sb("idxw", [128, 320]); idxwi = sb("idxwi", [128, 320], I32)
        same = sb("same", [128, 320])
        for ch in range(20):
            V(lambda e, ch=ch: e.tensor_tensor(out=cmpb[:], in0=pend.t[:, :].unsqueeze(1).broadcast_to([128, 16, 64]),
                                               in1=bc_last(bvals[:, ch * 16:(ch + 1) * 16], 64), op=ALU.is_le), [pend, bvals], [cmpb])
            V(lambda e, ch=ch: e.tensor_reduce(out=blk[:, ch * 16:(ch + 1) * 16], in_=cmpb[:], axis=AX.X, op=ALU.add), [cmpb], [blk])
        V(lambda e: e.tensor_scalar(out=blk[:], in0=blk[:], scalar1=63.0, scalar2=None, op0=ALU.min), [blk], [blk])
        V(lambda e: e.tensor_scalar(out=idxw[:], in0=blk[:], scalar1=128.0, scalar2=pcol[:, 0:1], op0=ALU.mult, op1=ALU.add), [blk, pcol], [idxw])
        NSL_ = debug.get('nsl', 3)
        if debug.get("wskip", True):
            V(lambda e: e.tensor_tensor(out=same[:, NSL_:320], in0=blk[:, NSL_:320], in1=blk[:, 0:320 - NSL_], op=ALU.is_equal), [blk], [same])
            V(lambda e: e.scalar_tensor_tensor(out=idxw[:, NSL_:320], in0=same[:, NSL_:320], scalar=1.0e6, in1=idxw[:, NSL_:320],
                                               op0=ALU.mult, op1=ALU.add), [same, idxw], [idxw])
        V(lambda e: e.tensor_copy(out=idxwi[:], in_=idxw[:]), [idxw], [idxwi])
        P.barrier()
        NSL = debug.get('nsl', 3)
        slot3 = [slot[0], slot[1]] + [sb("wslotC%d" % i, [128, 6144], BF16) for i in range(NSL - 2)]
        st_slot3 = [st_slot[0], st_slot[1]] + [P.stream("slotC%d" % i) for i in range(NSL - 2)]
        tw_all = sb("tw_all", [128, 320, 4]); twi_all = sb("twi_all", [128, 320], I32)
        P.dma('sp', P.stream("tw_all"), lambda e: e.dma_start(out=tw_all[:], in_=btw_d.rearrange("(b p) c -> p b c", p=128)), writes=[tw_all])
        V(lambda e: e.tensor_copy(out=twi_all[:], in_=tw_all[:, :, 0]), [tw_all], [twi_all])
        NXG = 4
        xg = [sb("xg%d" % i, [128, D], BF16) for i in range(NXG)]; st_xg = [P.stream("xg%d" % i) for i in range(NXG)]
        xgT = [sb("xgT%d" % i, [128, 8, 128], BF16) for i in range(3)]
        sgb = [sb("sgb%d" % i, [128, 256], BF16) for i in range(2)]; actb = [sb("actb%d" % i, [128, 256], BF16) for i in range(2)]
        yr = [sb("yr%d" % i, [128, D], BF16) for i in range(2)]; st_yr = [P.stream("yr%d" % i) for i in range(2)]
        NB = debug.get("nblk", 320)
        PTs = [[PS[0], PS[1]], [PS[6], PS[7]]]
        PGU = [PS[2], PS[3]]
        PD = [PS[4], PS[5]]

        def wviews(b):
            sl_ = slot3[b % NSL]
            return (sl_.t[:, 0:2048].rearrange("p (c h) -> p c h", c=8), sl_.t[:, 2048:4096].rearrange("p (c h) -> p c h", c=8),
                    sl_.t[:, 4096:6144].rearrange("p (c d) -> p c d", c=2), sl_)

        def S0a(b):
            x3 = b % NXG
            P.dma('pool', st_xg[x3], lambda e: e.indirect_dma_start(
                out=xg[x3][:, :], out_offset=None, in_=Hs_d[:, :], in_offset=IOA(ap=twi_all[:, b:b + 1], axis=0),
                bounds_check=breg(e, S), oob_is_err=False), reads=[twi_all], writes=[xg[x3]])

        def S0b(b):
            w3 = b % NSL
            P.dma('pool', st_slot3[w3], lambda e: e.indirect_dma_start(
                out=slot3[w3].t[:, :], out_offset=None, in_=wbf_d[:, :],
                in_offset=IOA(ap=idxwi[:, b:b + 1], axis=0), bounds_check=breg(e, 8191), oob_is_err=False),
                reads=[idxwi], writes=[slot3[w3]])

        def S1(b):
            x3 = b % 3
            xq = b % NXG
            pt0, pt1 = PTs[b % 2]
            for dc in range(8):
                pk = pt0 if dc < 4 else pt1
                P.op('pe', lambda e, pk=pk, dc=dc: e.matmul(pk[:, (dc % 4) * 128:(dc % 4 + 1) * 128], xg[xq][:, dc * 128:(dc + 1) * 128],
                                                          identb[:, :], start=True, stop=True), reads=[xg[xq], identb], writes=[pk])
            A(lambda e: e.activation(out=xgT[x3][:, 0:4, :], in_=pt0[:, :].rearrange("p (c r) -> p c r", c=4), func=AF.Copy),
              [pt0], [xgT[x3]])
            V(lambda e: e.tensor_copy(out=xgT[x3][:, 4:8, :], in_=pt1[:, :].rearrange("p (c r) -> p c r", c=4)), [pt1], [xgT[x3]])

        def S2(b):
            x3, a2 = b % 3, b % 2
            sg_v, su_v, sd_v, sl_ = wviews(b)
            pg, pu = PGU
            for pk, wv_ in ((pg, sg_v), (pu, su_v)):
                for hb in range(2):
                    for kc in range(8):
                        P.op('pe', lambda e, pk=pk, wv_=wv_, hb=hb, kc=kc: e.matmul(
                            pk[:, hb * 128:(hb + 1) * 128], wv_[:, kc, hb * 128:(hb + 1) * 128], xgT[x3][:, kc, :],
                            start=(kc == 0), stop=(kc == 7)), reads=[sl_, xgT[x3]], writes=[pk])
            A(lambda e: e.activation(out=sgb[a2][:], in_=pg[:, 0:256], func=AF.Silu), [pg], [sgb[a2]])
            V(lambda e: e.tensor_tensor(out=actb[a2][:], in0=sgb[a2][:], in1=pu[:, 0:256], op=ALU.mult), [sgb[a2], pu], [actb[a2]])

        def S3(b):
            a2 = b % 2
            sg_v, su_v, sd_v, sl_ = wviews(b)
            pd0, pd1 = PD
            for half, pk in ((0, pd0), (1, pd1)):
                for hb in range(2):
                    P.op('pe', lambda e, pk=pk, half=half, hb=hb: e.matmul(
                        pk[:, :], actb[a2][:, hb * 128:(hb + 1) * 128], sd_v[:, hb, half * 512:(half + 1) * 512],
                        start=(hb == 0), stop=(hb == 1)), reads=[actb[a2], sl_], writes=[pk])
            A(lambda e: e.activation(out=yr[a2][:, 0:512], in_=pd0[:, :], func=AF.Copy, scale=tw_all[:, b, 1:2]), [pd0, tw_all], [yr[a2]])
            V(lambda e: e.tensor_scalar(out=yr[a2][:, 512:1024], in0=pd1[:, :], scalar1=tw_all[:, b, 1:2], scalar2=None, op0=ALU.mult),
              [pd1, tw_all], [yr[a2]])
            P.dma('sp', st_yr[a2], lambda e: e.dma_start(out=yrows_d[b * 128:(b + 1) * 128, :], in_=yr[a2][:]), reads=[yr[a2]])

        for b in range(min(NXG, NB)):
            S0a(b)
        for b in range(min(NSL, NB)):
            S0b(b)
        for i in range(-2, NB):
            if 0 <= i + 2 < NB:
                S1(i + 2)
            if 0 <= i + 1 < NB:
                S2(i + 1)
            if 0 <= i:
                S3(i)
            if NXG <= i + 2 + NXG < NB:
                S0a(i + 2 + NXG)
            if NSL <= i + NSL < NB:
                S0b(i + NSL)
        P.barrier()
        LN_POOL[0] = False
        load_ln(ln2g_d, ln2b_d)
        NYG = 3
        yg = [sb("yg%d" % i, [128, D], BF16) for i in range(NYG)]; st_yg = [P.stream("yg%d" % i) for i in range(NYG)]
        for T_ in range(32):
            b2 = T_ % 2
            P.dma('sp', st_x[b2], lambda e, b2=b2, T_=T_: e.dma_start(out=rt[b2][:], in_=Zs_d[T_ * 128:(T_ + 1) * 128, :]), writes=[rt[b2]])
            pa0, pa1 = PS[2 * b2], PS[2 * b2 + 1]
            for k in range(8):
                j = (T_ * 8 + k) % NYG
                P.dma('pool', st_yg[j], lambda e, j=j, T_=T_, k=k: e.indirect_dma_start(
                    out=yg[j][:, :], out_offset=None, in_=yrows_d[:, :], in_offset=IOA(ap=d8i[:, T_, k:k + 1], axis=0),
                    bounds_check=breg(e, 40959), oob_is_err=False), reads=[d8i], writes=[yg[j]])
                for hf, pk in ((0, pa0), (1, pa1)):
                    P.op('pe', lambda e, j=j, hf=hf, pk=pk, k=k: e.matmul(pk[:, :], identb[:, :], yg[j][:, hf * 512:(hf + 1) * 512],
                                                                         start=(k == 0), stop=(k == 7)), reads=[identb, yg[j]], writes=[pk])
            for hf, pk in ((0, pa0), (1, pa1)):
                V(lambda e, b2=b2, hf=hf, pk=pk: e.tensor_tensor(out=rt[b2][:, hf * 512:(hf + 1) * 512], in0=rt[b2][:, hf * 512:(hf + 1) * 512],
                                                              in1=pk[:, :], op=ALU.add), [rt[b2], pk], [rt[b2]])
            layer_norm(rt[b2], xt[b2])
            P.dma('sp', st_out[b2], lambda e, b2=b2, T_=T_: e.dma_start(out=out_d[T_ * 128:(T_ + 1) * 128, :], in_=xt[b2][:]), reads=[xt[b2]])

    stages = debug.get('stages', 'F1F2F3F4F5')
    def F1(q, acc, hT, pTb, acc_v, hT_v, pT_v):
        t0 = q * QT
        if not RES:
            load_big(w_out_d)
        if not (debug.get('skipdma', 0) & 8):
            load_ln(ln1g_d, ln1b_d)
        pv = pT_d.rearrange("(c p) t -> p c t", p=128)
        if not (debug.get('skipdma', 0) & 16):
          P.dma('pool', st_p, lambda e, t0=t0: e.dma_start(out=pT_v, in_=pv[:, :, t0:t0 + QT]), writes=[pTb])
        for tt in range(NTT):
            tok = t0 + tt * 128
            b = tt % 2
            P.dma('sp', st_x[b], lambda e, b=b, tok=tok: e.dma_start(out=xt[b][:], in_=x_d[tok:tok + 128, :]),
                  writes=[xt[b]])
            for h in range(2):
                for kc in range(8):
                    P.op('pe', lambda e, h=h, kc=kc, tok=tok: e.matmul(
                        PS[h][:, :], mixT[:, kc, tok:tok + 128],
                        (wout_t[:, kc, h * 512:(h + 1) * 512] if RES else slot_big(h)[:, kc, :]),
                        start=(kc == 0), stop=(kc == 7)), reads=[mixT, (wout_t if RES else slot[h])], writes=[PS[h]])
                P.op('dve', lambda e, h=h, b=b: e.scalar_tensor_tensor(
                    out=rt[b][:, h * 512:(h + 1) * 512], in0=xt[b][:, h * 512:(h + 1) * 512], scalar=ALPHA,
                    in1=PS[h][:, :], op0=ALU.mult, op1=ALU.add), reads=[xt[b], PS[h]], writes=[rt[b]])
            lvl = debug.get('f1lvl', 3)
            if lvl >= 2:
                layer_norm(rt[b], xt[b])
            if SPARSE:
                P.dma('pool', st_hs[b], lambda e, b=b, tok=tok: e.dma_start(out=Hs_d[tok:tok + 128, :], in_=xt[b][:]), reads=[xt[b]])
            if lvl == 1:
                P.dma('sp', st_out[b], lambda e, b=b, tok=tok: e.dma_start(out=out_d[tok:tok + 128, :], in_=rt[b][:]), reads=[rt[b]])
            if lvl == 2:
                P.dma('sp', st_out[b], lambda e, b=b, tok=tok: e.dma_start(out=out_d[tok:tok + 128, :], in_=xt[b][:]), reads=[xt[b]])
            if tt > 0:
                f1_tail(tt - 1, acc, hT, acc_v, hT_v)
        f1_tail(NTT - 1, acc, hT, acc_v, hT_v)

    def f1_tail(tt, acc, hT, acc_v, hT_v):
        b = tt % 2
        for g4 in range(2):
            pb = PS[2 + g4]
            for j in range(4):
                dc = g4 * 4 + j
                P.op('pe', lambda e, pb=pb, j=j, dc=dc, b=b: e.transpose(
                    pb[:, j * 128:(j + 1) * 128], xt[b][:, dc * 128:(dc + 1) * 128], ident[:]),
                    reads=[xt[b], ident], writes=[pb])
            P.op('act', lambda e, pb=pb, g4=g4, tt=tt: e.activation(
                out=hT_v[:, g4 * 4:(g4 + 1) * 4, tt * 128:(tt + 1) * 128],
                in_=pb[:, :].rearrange("p (c t) -> p c t", c=4), func=AF.Copy), reads=[pb], writes=[hT, pb])
            P.op('dve', lambda e, pb=pb, g4=g4, tt=tt: e.tensor_scalar(
                out=acc_v[:, g4 * 4:(g4 + 1) * 4, tt * 128:(tt + 1) * 128],
                in0=pb[:, :].rearrange("p (c t) -> p c t", c=4), scalar1=ALPHA, scalar2=None,
                op0=ALU.mult), reads=[pb], writes=[acc])

    def F2(q, acc, hT, pTb, acc_v, hT_v, pT_v):
        t0 = q * QT
        for tt in range(NTT if 'F2' in stages else 0):
            pl = PS[4 + tt % 2]
            for kc in range(8):
                P.op('pe', lambda e, pl=pl, kc=kc, tt=tt: e.matmul(
                    pl[:, 0:64], acc_v[:, kc, tt * 128:(tt + 1) * 128], wr[:, kc, :],
                    start=(kc == 0), stop=(kc == 7)), reads=[acc, wr], writes=[pl])
            P.op('act', lambda e, pl=pl: e.activation(out=sc[:], in_=pl[:, 0:64], func=AF.Sigmoid, scale=1.0 / ALPHA),
                 reads=[pl], writes=[sc])
            P.op('dve', lambda e: e.tensor_tensor(out=sel[:], in0=sc[:], in1=rbias[:], op=ALU.add),
                 reads=[sc, rbias], writes=[sel])
            sel3 = sel.t[:, :].rearrange("p (g k) -> p g k", g=8)
            P.op('dve', lambda e: e.tensor_reduce(out=m1[:], in_=sel3, axis=AX.X, op=ALU.max),
                 reads=[sel], writes=[m1])
            P.op('dve', lambda e: e.tensor_tensor(out=eq.t[:, :].rearrange("p (g k) -> p g k", g=8), in0=sel3,
                                                  in1=bc_last(m1[:, :], 8), op=ALU.is_equal),
                 reads=[sel, m1], writes=[eq])
            P.op('dve', lambda e: e.scalar_tensor_tensor(out=sel2[:], in0=eq[:], scalar=-1e9, in1=sel[:],
                                                         op0=ALU.mult, op1=ALU.add),
                 reads=[eq, sel], writes=[sel2])
            P.op('dve', lambda e: e.tensor_reduce(out=m2[:], in_=sel2.t[:, :].rearrange("p (g k) -> p g k", g=8),
                                                  axis=AX.X, op=ALU.max), reads=[sel2], writes=[m2])
            P.op('dve', lambda e: e.tensor_tensor(out=gs[:], in0=m1[:], in1=m2[:], op=ALU.add),
                 reads=[m1, m2], writes=[gs])
            P.op('dve', lambda e: e.max(out=top8[:], in_=gs[:]), reads=[gs], writes=[top8])
            P.op('dve', lambda e: e.tensor_scalar(out=pen[:], in0=gs[:], scalar1=top8[:, 3:4], scalar2=1e9,
                                                  op0=ALU.is_ge, op1=ALU.mult), reads=[gs, top8], writes=[pen])
            P.op('dve', lambda e: e.tensor_scalar(out=pen[:], in0=pen[:], scalar1=-1e9, scalar2=None, op0=ALU.add),
                 reads=[pen], writes=[pen])
            P.op('dve', lambda e: e.tensor_tensor(out=selm.t[:, :].rearrange("p (g k) -> p g k", g=8), in0=sel3,
                                                  in1=bc_last(pen[:, :], 8), op=ALU.add),
                 reads=[sel, pen], writes=[selm])
            P.op('dve', lambda e: e.max(out=top8[:], in_=selm[:]), reads=[selm], writes=[top8])
            P.op('dve', lambda e: e.tensor_scalar(out=msk[:], in0=selm[:], scalar1=top8[:, 7:8], scalar2=None,
                                                  op0=ALU.is_ge), reads=[selm, top8], writes=[msk])
            P.op('dve', lambda e: e.tensor_tensor(out=wsel[:], in0=sc[:], in1=msk[:], op=ALU.mult),
                 reads=[sc, msk], writes=[wsel])
            P.op('dve', lambda e: e.tensor_reduce(out=ssum[:], in_=wsel[:], axis=AX.X, op=ALU.add),
                 reads=[wsel], writes=[ssum])
            P.op('dve', lambda e: e.reciprocal(out=ssum[:], in_=ssum[:]), reads=[ssum], writes=[ssum])
            P.op('dve', lambda e: e.tensor_scalar(out=wt[:], in0=wsel[:], scalar1=ssum[:, 0:1], scalar2=2.5,
                                                  op0=ALU.mult, op1=ALU.mult), reads=[wsel, ssum], writes=[wt])
            if SPARSE:
                T_ = q * NTT + tt
                P.op('dve', lambda e, T_=T_: e.tensor_copy(out=mskb[:, T_ % NTT, :], in_=msk[:]), reads=[msk], writes=[mskb])
                P.op('dve', lambda e, T_=T_: e.tensor_copy(out=wt_all[:, T_, :], in_=wt[:]), reads=[wt], writes=[wt_all])
            else:
                pw = PS[6]
                P.op('pe', lambda e, pw=pw: e.transpose(pw[0:64, 0:128], wt[:, :], ident[:]),
                     reads=[wt, ident], writes=[pw])
                P.op('act', lambda e, pw=pw, tt=tt: e.activation(out=wtT_v[0:64, tt * 128:(tt + 1) * 128],
                                                                 in_=pw[0:64, 0:128], func=AF.Copy),
                     reads=[pw], writes=[wtT])
    def F2b(q):
        for tt in range(NTT if ('F2' in stages and SPARSE) else 0):
            T_ = q * NTT + tt
            pw = PS[6 + tt % 2]
            P.op('pe', lambda e, pw=pw, tt=tt: e.matmul(pw[:, 0:64], trib[:, :], mskb[:, tt, :], start=True, stop=True),
                 reads=[trib, mskb], writes=[pw])
            P.op('pe', lambda e, pw=pw, tt=tt: e.matmul(pw[:, 64:128], onesb[:, :], mskb[:, tt, :], start=True, stop=True),
                 reads=[onesb, mskb], writes=[pw])
            P.op('dve', lambda e, pw=pw, T_=T_: e.tensor_tensor(out=rank_all[:, T_, :], in0=pw[:, 0:64], in1=cum[:], op=ALU.add),
                 reads=[pw, cum], writes=[rank_all])
            P.op('dve', lambda e, pw=pw: e.tensor_tensor(out=cum[:], in0=pw[:, 64:128], in1=cum[:], op=ALU.add),
                 reads=[pw, cum], writes=[cum])

    def F345(q, acc, hT, pTb, acc_v, hT_v, pT_v):
        t0 = q * QT
        NE = debug.get('ne', 65) if 'F3' in stages else 0
        E0 = 64 if SPARSE else 0
        pending = (load_expert(E0, E0 % 2) if NE else None) if not RES else None
        for ei in range(E0, NE):
            s = ei % 2
            if RES:
                sg_v = wsh_t.t[:, 0:2048].rearrange("p (c h) -> p c h", c=8)
                su_v = wsh_t.t[:, 2048:4096].rearrange("p (c h) -> p c h", c=8)
                sd_v = wsh_t.t[:, 4096:6144].rearrange("p (c d) -> p c d", c=2)
                wtile = wsh_t
            else:
                sg_v, su_v, sd_v = pending
                wtile = slot[s]
                if ei + 1 < NE:
                    pending = load_expert(ei + 1, (ei + 1) % 2)
            if ei < 64:
                P.op('act', lambda e, s=s, ei=ei: e.activation(
                    out=oh[s][:], in_=identb[0:64, ei:ei + 1].broadcast_to([64, 128]), func=AF.Copy),
                    reads=[identb], writes=[oh[s]])
            for tb in range(NTB):
                tsl = slice(tb * 512, (tb + 1) * 512)
                if ei < 64:
                    P.op('pe', lambda e, s=s, tsl=tsl: e.matmul(PS[4][:, :], oh[s][:, :], wtT_v[0:64, tsl],
                                                                start=True, stop=True),
                         reads=[oh[s], wtT], writes=[PS[4]])
                for hb in range(2):
                    pg, pu = PS[hb], PS[2 + hb]
                    for kc in range(8):
                        P.op('pe', lambda e, pg=pg, kc=kc, hb=hb, tsl=tsl, sg_v=sg_v: e.matmul(
                            pg[:, :], sg_v[:, kc, hb * 128:(hb + 1) * 128], hT_v[:, kc, tsl],
                            start=(kc == 0), stop=(kc == 7)), reads=[wtile, hT], writes=[pg])
                    for kc in range(8):
                        P.op('pe', lambda e, pu=pu, kc=kc, hb=hb, tsl=tsl, su_v=su_v: e.matmul(
                            pu[:, :], su_v[:, kc, hb * 128:(hb + 1) * 128], hT_v[:, kc, tsl],
                            start=(kc == 0), stop=(kc == 7)), reads=[wtile, hT], writes=[pu])
                    P.op('act', lambda e, pg=pg, hb=hb: e.activation(out=sgt[hb][:], in_=pg[:, :], func=AF.Silu),
                         reads=[pg], writes=[sgt[hb]])
                    if ei < 64:
                        P.op('dve', lambda e, pu=pu, hb=hb: e.tensor_tensor(out=t1t[hb][:], in0=sgt[hb][:],
                                                                            in1=pu[:, :], op=ALU.mult),
                             reads=[sgt[hb], pu], writes=[t1t[hb]])
                        P.op('dve', lambda e, hb=hb, tb=tb: e.tensor_tensor(out=actw[tb][hb][:], in0=t1t[hb][:],
                                                                            in1=PS[4][:, :], op=ALU.mult),
                             reads=[t1t[hb], PS[4]], writes=[actw[tb][hb]])
                    else:
                        P.op('dve', lambda e, pu=pu, hb=hb, tb=tb: e.tensor_tensor(
                            out=actw[tb][hb][:], in0=sgt[hb][:], in1=pu[:, :], op=ALU.mult),
                            reads=[sgt[hb], pu], writes=[actw[tb][hb]])
                for dc in range(8):
                    po = PS[5 + dc % 3]
                    for hb in range(2):
                        P.op('pe', lambda e, po=po, hb=hb, dc=dc, tb=tb, sd_v=sd_v: e.matmul(
                            po[:, :], sd_v[:, hb, dc * 128:(dc + 1) * 128], actw[tb][hb][:, :],
                            start=(hb == 0), stop=(hb == 1)), reads=[wtile, actw[tb][hb]], writes=[po])
                    P.op('dve', lambda e, po=po, dc=dc, tsl=tsl: e.tensor_tensor(
                        out=acc_v[:, dc, tsl], in0=acc_v[:, dc, tsl], in1=po[:, :], op=ALU.add),
                        reads=[acc, po], writes=[acc])
        if 'F4' in stages:
            if not RES:
                load_big(wpg_d)
        for tb in range(NTB if 'F4' in stages else 0):
            tsl = slice(tb * 512, (tb + 1) * 512)
            for dc in range(8):
                pa, pbk = PS[dc % 2], PS[2 + dc % 2]
                for kc in range(2):
                    P.op('pe', lambda e, pa=pa, kc=kc, dc=dc, tsl=tsl: e.matmul(
                        pa[:, :], wple[:, kc, dc * 128:(dc + 1) * 128], pT_v[:, kc, tsl],
                        start=(kc == 0), stop=(kc == 1)), reads=[wple, pTb], writes=[pa])
                hh = dc // 4
                for kc in range(8):
                    P.op('pe', lambda e, pbk=pbk, kc=kc, dc=dc, tsl=tsl, hh=hh: e.matmul(
                        pbk[:, :], (wpg_t[:, kc, dc * 128:(dc + 1) * 128] if RES else slot_big(hh)[:, kc, (dc % 4) * 128:(dc % 4 + 1) * 128]),
                        hT_v[:, kc, tsl],
                        start=(kc == 0), stop=(kc == 7)), reads=[(wpg_t if RES else slot[hh]), hT], writes=[pbk])
                sg_ = sig[dc % 2]
                P.op('act', lambda e, pbk=pbk, sg_=sg_: e.activation(out=sg_[:], in_=pbk[:, :], func=AF.Sigmoid),
                     reads=[pbk], writes=[sg_])
                P.op('dve', lambda e, pa=pa, sg_=sg_: e.tensor_tensor(out=sg_[:], in0=sg_[:], in1=pa[:, :], op=ALU.mult),
                     reads=[sg_, pa], writes=[sg_])
                P.op('dve', lambda e, sg_=sg_, dc=dc, tsl=tsl: e.tensor_tensor(
                    out=acc_v[:, dc, tsl], in0=acc_v[:, dc, tsl], in1=sg_[:], op=ALU.add),
                    reads=[acc, sg_], writes=[acc])
        if 'F5' in stages and not SPARSE:
            load_ln(ln2g_d, ln2b_d)
        for tt in range(NTT if 'F5' in stages else 0):
            b = tt % 2
            tok = t0 + tt * 128
            for g4 in range(2):
                pb = PS[2 * b + g4]
                for j in range(4):
                    dc = g4 * 4 + j
                    P.op('pe', lambda e, pb=pb, j=j, dc=dc, tt=tt: e.transpose(
                        pb[:, j * 128:(j + 1) * 128], acc_v[:, dc, tt * 128:(tt + 1) * 128], ident[:]),
                        reads=[acc, ident], writes=[pb])
                P.op('act', lambda e, pb=pb, g4=g4, b=b: e.activation(out=rt[b][:, g4 * 512:(g4 + 1) * 512],
                                                                     in_=pb[:, :], func=AF.Copy),
                     reads=[pb], writes=[rt[b]])
            if SPARSE:
                P.dma('sp', st_zs[b], lambda e, b=b, tok=tok: e.dma_start(out=Zs_d[tok:tok + 128, :], in_=rt[b][:]), reads=[rt[b]])
            else:
                layer_norm(rt[b], xt[b])
                P.dma('sp', st_out[b], lambda e, b=b, tok=tok: e.dma_start(out=out_d[tok:tok + 128, :], in_=xt[b][:]),
                      reads=[xt[b]])

    bufs = [(acc, hT, pTb, acc_v, hT_v, pT_v)]
    nq_ = debug.get('nq', NQ)
    for q in range(nq_):
        F1(q, *bufs[0])
        F2(q, *bufs[0])
        F345(q, *bufs[0])
        F2b(q)
    P.barrier()
    if SPARSE:
        sparse_moe()

    P.ops['sp'].append(([(st_out[0], P.cnt[st_out[0]]), (st_out[1], P.cnt[st_out[1]])], None, None))
    flush()
    esB.close()
    esA.close()
    esP.close()
    es_mix.close()
    es.close()
    return nc


def make_consts():
    c = {}
    c["c_ident"] = np.eye(128, dtype=np.float32)
    selF = np.zeros((128, 4, 8, 128), np.float32)
    for a in range(2):
        for q in range(4):
            for j in range(8):
                for cin in range(16):
                    selF[64 * a + q * 16 + cin, q, j, j * 16 + cin] = 1.0
    c["c_selF2"] = selF.reshape(128, 4096)
    selU = np.zeros((128, 4, 8, 64), np.float32)
    for i in range(8):
        for q in range(4):
            for co in range(16):
                selU[i * 16 + co, q, i, q * 16 + co] = 1.0
    c["c_selU2"] = selU.reshape(128, 2048)
    jj = np.arange(128) // 16
    c["c_maskK"] = (jj[None, :] >= jj[:, None]).astype(np.float32)
    kv = [7 - j for j in range(8)] + [i - 7 for i in range(8)] + [i + 1 for i in range(8)] + [8 * (1 << s) for s in range(9)]
    c["c_kvals"] = np.tile(np.asarray(kv, np.float32)[None, :], (128, 1))
    half = (np.arange(128) >= 64).astype(np.float32)
    c["c_half"] = np.stack([1.0 - half, half, 2.0 * half - 1.0, -half], axis=1).astype(np.float32)
    ki = np.arange(128)[:, None]; qi = np.arange(128)[None, :]
    NEG = -30000.0
    c["c_maskb"] = np.concatenate([np.where(ki >= qi, 0.0, NEG), np.where(ki <= qi, 0.0, NEG)], axis=1).astype(np.float32)
    t = np.arange(S)
    a_t = (t // 64).astype(np.float32); b_t = (t % 64).astype(np.float32)
    slopes = np.asarray([2.0 ** (-(h + 1)) for h in range(8)], np.float32)
    qaug = np.zeros((8, 4, S), np.float32)
    for h in range(8):
        qaug[h, 0] = -slopes[h] * 64.0 * a_t
        qaug[h, 1] = -slopes[h] * b_t
        qaug[h, 2] = slopes[h]
        qaug[h, 3] = slopes[h]
    c["c_qaug"] = qaug
    c["c_kaug"] = np.stack([np.ones(S, np.float32), np.ones(S, np.float32), 64.0 * a_t, b_t], axis=0).astype(np.float32)
    lsel = np.zeros((65, 128), np.float32); lsel[64, :] = 1.0
    c["c_lsel"] = lsel
    place = np.zeros((65, 2, 128), np.float32)
    for e in range(64):
        place[e, 0, e] = 1.0
        place[e, 1, 64 + e] = 1.0
    c["c_place"] = place
    c["c_perm"] = np.roll(np.eye(128, dtype=np.float32), 64, axis=1)
    c["c_tri"] = (np.arange(128)[:, None] < np.arange(128)[None, :]).astype(np.float32)
    c["c_tokid"] = (np.arange(32)[None, :] * 128 + np.arange(128)[:, None]).astype(np.float32)
    c["c_pcol"] = np.arange(128, dtype=np.float32)[:, None].copy()
    c["c_bvals"] = np.tile((np.arange(320, dtype=np.float32) * 128.0)[None, :], (128, 1))
    c["c_prefill"] = np.tile(np.asarray([float(S), 0.0, 0.0, 0.0], np.float32), (128, 320))
    return c


def core_inputs(inputs, b, consts):
    g = lambda k: np.asarray(inputs[k])[0]
    m = dict(consts)
    x = np.asarray(inputs["x"])
    m["x"] = np.ascontiguousarray(x[b])
    m["xT"] = np.ascontiguousarray(x[b].T)
    m["pT"] = np.ascontiguousarray(g("p")[b].T)
    for k in ("w_in", "w_out", "ln1_g", "ln1_b", "ln2_g", "ln2_b", "w_router", "router_bias",
              "ws_gate", "ws_up", "ws_down", "w_ple", "w_ple_gate", "w_glu"):
        m[k] = np.ascontiguousarray(g(k))
    if "wg2" not in _WCACHE:
        _WCACHE["wg2"] = np.ascontiguousarray(g("w_gate").reshape(64, 8, 128, 256).transpose(0, 2, 1, 3).reshape(8192, 2048))
        _WCACHE["wu2"] = np.ascontiguousarray(g("w_up").reshape(64, 8, 128, 256).transpose(0, 2, 1, 3).reshape(8192, 2048))
        _WCACHE["wd2"] = np.ascontiguousarray(g("w_down").reshape(64, 2, 128, 1024).transpose(0, 2, 1, 3).reshape(8192, 2048))
    m["wg2"] = _WCACHE["wg2"]; m["wu2"] = _WCACHE["wu2"]; m["wd2"] = _WCACHE["wd2"]
    m["lamre2"] = np.ascontiguousarray(np.tile(g("lam_re").T, (2, 1)))
    m["lamim2"] = np.ascontiguousarray(np.tile(g("lam_im").T, (2, 1)))
    m["logdt2"] = np.ascontiguousarray(np.tile(g("log_dt")[None, :], (128, 1)))
    m["bre2"] = np.ascontiguousarray(np.tile(g("b_re").transpose(1, 0, 2), (2, 1, 1)))
    m["bim2"] = np.ascontiguousarray(np.tile(g("b_im").transpose(1, 0, 2), (2, 1, 1)))
    m["cre2"] = np.ascontiguousarray(np.tile(g("c_re").transpose(2, 0, 1), (2, 1, 1)))
    m["cim2"] = np.ascontiguousarray(np.tile(g("c_im").transpose(2, 0, 1), (2, 1, 1)))
    m["dsk2"] = np.ascontiguousarray(np.tile(g("d_skip").T, (8, 1)))
    m["bglu2"] = np.ascontiguousarray(g("b_glu").reshape(4, 128).T)
    return m


_NC = [None]
_WCACHE = {}


def kernel(**inputs):
    if _NC[0] is None:
        _NC[0] = build()
    nc = _NC[0]
    consts = make_consts()
    _WCACHE.clear()
    in_maps = [core_inputs(inputs, b, consts) for b in range(8)]
    res = run_bass_kernel_spmd(nc, in_maps, core_ids=list(range(8)))
    return np.stack([r["out"] for r in res.results], axis=0).astype(np.float32)
```

```python
import math
from contextlib import ExitStack
import numpy as np
import ml_dtypes
import concourse.bass as bass
import concourse.mybir as mybir
from concourse.bass_utils import run_bass_kernel_spmd

F32 = mybir.dt.float32
BF16 = mybir.dt.bfloat16
I32 = mybir.dt.int32
AF = mybir.ActivationFunctionType
ALU = mybir.AluOpType
AX = mybir.AxisListType

S = 4096
D = 1024
ALPHA = 2.0 ** 0.25
EPS = 1e-5
TWO_PI_S = 2.0 * math.pi * (1.0 - 2e-6)
NQ = 8
QT = S // NQ
NTT = QT // 128
NTB = QT // 512
ENGS = ['pe', 'act', 'dve', 'pool', 'sp']


class TK:
    def __init__(self):
        self.w = None
        self.r = {}


class Tl:
    def __init__(self, t, tk=None):
        self.t = t
        self.tk = tk or TK()

    def __getitem__(self, idx):
        return self.t[idx]


class Prog:
    def __init__(self, nc, es):
        self.nc = nc
        self.es = es
        self.ops = {e: [] for e in ENGS}
        self.cnt = {e: 0 for e in ENGS}
        self.seen = {e: {} for e in ENGS}
        self.sem = {}
        for e in ENGS:
            self.sem[e] = es.enter_context(nc.semaphore("s_" + e))
        self.out_streams = []

    def stream(self, name):
        self.sem[name] = self.es.enter_context(self.nc.semaphore("d_" + name))
        self.cnt[name] = 0
        return name

    def _waits(self, eng, reads, writes):
        deps = {}

        def add(d):
            if d is None:
                return
            f, c = d
            if c > deps.get(f, 0):
                deps[f] = c
        for t in reads:
            add(t.tk.w)
        for t in writes:
            add(t.tk.w)
            for f, c in t.tk.r.items():
                add((f, c))
        waits = []
        for f, c in deps.items():
            if f == eng and eng == 'pe':
                continue
            if c > self.seen[eng].get(f, 0):
                waits.append((f, c))
                self.seen[eng][f] = c
        return waits

    def op(self, eng, fn, reads=(), writes=()):
        waits = self._waits(eng, reads, writes)
        self.cnt[eng] += 1
        n = self.cnt[eng]
        self.ops[eng].append((waits, fn, (eng, 1)))
        for t in reads:
            t.tk.r[eng] = n
        for t in writes:
            t.tk.w = (eng, n)
            t.tk.r = {}

    def dma(self, q, st, fn, reads=(), writes=()):
        waits = self._waits(q, reads, writes)
        self.cnt[st] += 16
        n = self.cnt[st]
        self.ops[q].append((waits, fn, (st, 16)))
        for t in reads:
            t.tk.r[st] = n
        for t in writes:
            t.tk.w = (st, n)
            t.tk.r = {}

    def barrier(self):
        snap = dict(self.cnt)
        for e in ENGS:
            waits = []
            for f, c in snap.items():
                if f != e and c > self.seen[e].get(f, 0):
                    waits.append((f, c))
                    self.seen[e][f] = c
            self.ops[e].append((waits, None, None))

    def emit(self, eobj, eng):
        for waits, fn, inc in self.ops[eng]:
            for f, c in waits:
                eobj.wait_ge(self.sem[f], c)
            if fn is not None:
                ins = fn(eobj)
                ins.then_inc(self.sem[inc[0]], inc[1])


def bc_last(ap, n):
    sh = list(ap.shape)
    return ap.unsqueeze(len(sh)).broadcast_to(sh + [n])


def build(debug=None):
    debug = debug or {}
    nc = bass.Bass("TRN2", target_bir_lowering=False)
    es = ExitStack()
    P = Prog(nc, es)

    def din(name, shape, dt=F32):
        return nc.dram_tensor(name, list(shape), dt, kind="ExternalInput").ap()

    xT_d = din("xT", [D, S]) if not debug.get("ffn_only") else None
    x_d = din("x", [S, D])
    pT_d = din("pT", [256, S])
    w_in_d = din("w_in", [D, 2048]) if not debug.get("ffn_only") else None
    w_out_d = din("w_out", [D, D])
    ln1g_d = din("ln1_g", [D]); ln1b_d = din("ln1_b", [D])
    ln2g_d = din("ln2_g", [D]); ln2b_d = din("ln2_b", [D])
    wr_d = din("w_router", [D, 64]); rb_d = din("router_bias", [64])
    SPARSE = debug.get("sparse", True)
    if SPARSE:
        wg2_d = din("wg2", [8192, 2048]); wu2_d = din("wu2", [8192, 2048]); wd2_d = din("wd2", [8192, 2048])
        c_tri_d = din("c_tri", [128, 128]); c_tokid_d = din("c_tokid", [128, 32]); c_pcol_d = din("c_pcol", [128, 1])
        c_bvals_d = din("c_bvals", [128, 320]); c_prefill_d = din("c_prefill", [128, 1280])
        Hs_d = nc.dram_tensor("Hs", [S + 1, D], BF16, kind="Internal").ap()
        Zs_d = nc.dram_tensor("Zs", [S, D], F32, kind="Internal").ap()
        btw_d = nc.dram_tensor("btw", [40960, 4], F32, kind="Internal").ap()
        yrows_d = nc.dram_tensor("yrows", [40960, D], BF16, kind="Internal").ap()
        wbf_d = nc.dram_tensor("wbf", [8192, 6144], BF16, kind="Internal").ap()
        wg_d = wu_d = wd_d = None
    else:
        wg_d = din("w_gate", [64, D, 256]); wu_d = din("w_up", [64, D, 256]); wd_d = din("w_down", [64, 256, D])
    wsg_d = din("ws_gate", [D, 256]); wsu_d = din("ws_up", [D, 256]); wsd_d = din("ws_down", [256, D])
    wple_d = din("w_ple", [256, D]); wpg_d = din("w_ple_gate", [D, D])
    ident_d = din("c_ident", [128, 128])
    out_d = nc.dram_tensor("out", [S, D], F32, kind="ExternalOutput").ap()
    mixT_dbg = din("mixT_dbg", [D, S]) if debug.get("ffn_only") else None
    if not debug.get("ffn_only"):
        lamre_d = din("lamre2", [128, 32]); lamim_d = din("lamim2", [128, 32]); logdt_d = din("logdt2", [128, 32])
        bre_d = din("bre2", [128, 32, 16]); bim_d = din("bim2", [128, 32, 16])
        cre_d = din("cre2", [128, 32, 16]); cim_d = din("cim2", [128, 32, 16]); dsk_d = din("dsk2", [128, 32])
        wglu_d = din("w_glu", [512, 512]); bglu_d = din("bglu2", [128, 4])
        c_selF_d = din("c_selF2", [128, 4096]); c_selU_d = din("c_selU2", [128, 2048]); c_maskK_d = din("c_maskK", [128, 128])
        c_kvals_d = din("c_kvals", [128, 33]); c_half_d = din("c_half", [128, 4]); c_maskb_d = din("c_maskb", [128, 256])
        c_qaug_d = din("c_qaug", [8, 4, S]); c_kaug_d = din("c_kaug", [4, S])
        c_lsel_d = din("c_lsel", [65, 128]); c_place_d = din("c_place", [65, 2, 128])
        c_perm_d = din("c_perm", [128, 128])
    mixout_d = (nc.dram_tensor("mixout", [D, S], F32, kind="ExternalOutput").ap() if debug.get("mix_only") else None)

    def sb(name, shape, dt=F32):
        return Tl(es.enter_context(nc.sbuf_tensor(name, list(shape), dt)))

    def ps(name):
        return Tl(es.enter_context(nc.psum_tensor(name, [128, 512], F32)))

    PS = [ps("ps%d" % i) for i in range(8)]

    ident = sb("ident", [128, 128], F32)
    identb = sb("identb", [128, 128], BF16)
    st_c = P.stream("const")
    P.dma('sp', st_c, lambda e: e.dma_start(out=ident[:], in_=ident_d[:, :]), writes=[ident])
    P.op('dve', lambda e: e.tensor_copy(out=identb[:], in_=ident[:]), reads=[ident], writes=[identb])

    IOA = bass.IndirectOffsetOnAxis
    if SPARSE:
        st_hs = [P.stream("hs0"), P.stream("hs1")]
        st_zs = [P.stream("zs0"), P.stream("zs1")]
        mskb = sb("mskb", [128, NTT, 64], BF16)
        cum = sb("cum", [128, 64], F32)
        trib = sb("trib", [128, 128], BF16); onesb = sb("onesb", [128, 128], BF16)
        P.dma('pool', P.stream("tri"), lambda e: e.dma_start(out=trib[:], in_=c_tri_d[:, :]), writes=[trib])
        P.op('dve', lambda e: e.memset(onesb[:], 1.0), writes=[onesb])
        P.op('dve', lambda e: e.memset(cum[:], 0.0), writes=[cum])

    es_mix = ExitStack()
    mixT = Tl(es_mix.enter_context(nc.sbuf_tensor("mixT", [128, 8, debug.get("s_alloc", S)], BF16)))

    _regs = {}

    def breg(e, val):
        if val not in _regs:
            _regs[val] = e.to_reg(val)
        return _regs[val]

    def flush():
        P.barrier()
        _regs.clear()
        with nc.Block() as block:
            @block.tensor
            def _(e):
                P.emit(e, 'pe')

            @block.scalar
            def _(e):
                P.emit(e, 'act')

            @block.vector
            def _(e):
                P.emit(e, 'dve')

            @block.gpsimd
            def _(e):
                P.emit(e, 'pool')

            @block.sync
            def _(e):
                P.emit(e, 'sp')
        for e_ in ENGS:
            P.ops[e_] = []

    _psk = [0]

    def nextps():
        _psk[0] += 1
        return PS[_psk[0] % 8]

    def V(fn, reads, writes):
        P.op('dve', fn, reads=reads, writes=writes)

    def A(fn, reads, writes):
        P.op('act', fn, reads=reads, writes=writes)

    def evac(k, out_ap, pb, out_tile, in_ap=None):
        src = pb[:, :] if in_ap is None else in_ap
        if k % 2 == 0:
            A(lambda e: e.activation(out=out_ap, in_=src, func=AF.Copy), [pb], [out_tile])
        else:
            V(lambda e: e.tensor_copy(out=out_ap, in_=src), [pb], [out_tile])

    mix_lo = Tl(mixT.t)
    mix_hi = Tl(mixT.t)
    wv_in = w_in_d.rearrange("(c p) n -> p c n", p=128) if w_in_d is not None else None
    xv = xT_d.rearrange("(c p) t -> p c t", p=128) if xT_d is not None else None

    def load_xT(stack):
        xT_ = Tl(stack.enter_context(nc.sbuf_tensor("xT_%d" % len(P.sem), [128, 8, S], BF16)))
        st_ = P.stream("xT%d" % len(P.sem))
        for c in range(8):
            P.dma('pool', st_, lambda e, c=c: e.dma_start(out=xT_[:, c, :], in_=xv[:, c, :]), writes=[xT_])
        return xT_

    if debug.get("ffn_only"):
        st_m = P.stream("mixdbg")
        mv = mixT_dbg.rearrange("(c p) t -> p c t", p=128)
        for c in range(8):
            P.dma('pool', st_m, lambda e, c=c: e.dma_start(out=mixT[:, c, :], in_=mv[:, c, 0:debug.get('s_alloc', S)]), writes=[mixT])
    else:
        do_ssm = debug.get("do_ssm", True)
        do_att = debug.get("do_att", True)
        if SPARSE:
            st_wst_in = P.stream("wst_in"); st_wst_out = P.stream("wst_out")
        if do_ssm:
            es1 = ExitStack()
            xT = load_xT(es1)
            winu = Tl(es1.enter_context(nc.sbuf_tensor("winu", [128, 8, 512], BF16)))
            P.dma('pool', P.stream("winu"), lambda e: e.dma_start(out=winu[:], in_=wv_in[:, :, 1536:2048]), writes=[winu])
            k = 0
            for gb in range(4):
                for tb in range(8):
                    pb = PS[k % 2]
                    for kc in range(8):
                        P.op('pe', lambda e, pb=pb, kc=kc, gb=gb, tb=tb: e.matmul(
                            pb[:, :], winu[:, kc, gb * 128:(gb + 1) * 128], xT[:, kc, tb * 512:(tb + 1) * 512],
                            start=(kc == 0), stop=(kc == 7)), reads=[winu, xT], writes=[pb])
                    evac(k, mixT[:, 4 + gb, tb * 512:(tb + 1) * 512], pb, mix_hi)
                    k += 1
            flush()
            es1.close()

            es2 = ExitStack()

            def sb2(name, shape, dt=F32):
                return Tl(es2.enter_context(nc.sbuf_tensor(name, list(shape), dt)))

            def ld(name, shape, src, dt=F32, q='sp'):
                t = sb2(name, shape, dt)
                P.dma(q, P.stream("l_" + name), lambda e: e.dma_start(out=t[:], in_=src), writes=[t])
                return t
            lamre = ld("lamre", [128, 32], lamre_d[:, :]); lamim = ld("lamim", [128, 32], lamim_d[:, :])
            logdt = ld("logdt", [128, 32], logdt_d[:, :])
            bre = ld("bre", [128, 32, 16], bre_d[:, :, :]); bim = ld("bim", [128, 32, 16], bim_d[:, :, :])
            cre = ld("cre", [128, 32, 16], cre_d[:, :, :]); cim = ld("cim", [128, 32, 16], cim_d[:, :, :])
            dsk = ld("dsk", [128, 32], dsk_d[:, :]); kvals = ld("kvals", [128, 33], c_kvals_d[:, :])
            halfc = ld("halfc", [128, 4], c_half_d[:, :]); maskK = ld("maskK", [128, 128], c_maskK_d[:, :])
            perm = ld("perm", [128, 128], c_perm_d[:, :])
            selF = ld("selF", [128, 4, 8, 128], c_selF_d.rearrange("p (a j m) -> p a j m", a=4, j=8), BF16, 'pool')
            selU = ld("selU", [128, 4, 8, 64], c_selU_d.rearrange("p (a j m) -> p a j m", a=4, j=8), BF16, 'pool')
            wglu = ld("wglu", [128, 4, 512], wglu_d.rearrange("(c p) n -> p c n", p=128), BF16, 'pool')
            bglu = ld("bglu", [128, 4], bglu_d[:, :])
            X1 = sb2("X1", [128, 32, 8]); X2 = sb2("X2", [128, 32, 8])
            Y1 = sb2("Y1", [128, 32, 16]); Y2 = sb2("Y2", [128, 32, 16])
            crt = sb2("crt", [128, 32, 9]); cst = sb2("cst", [128, 32, 9]); csn = sb2("csn", [128, 32, 9])

            es3 = ExitStack()

            def sb3(name, shape, dt=F32):
                return Tl(es3.enter_context(nc.sbuf_tensor(name, list(shape), dt)))
            dt_t = sb3("dt_t", [128, 32]); a_t = sb3("a_t", [128, 32]); u1 = sb3("u1", [128, 32]); s32 = sb3("s32", [128, 32])
            i32 = sb3("i32", [128, 32], I32)
            T1 = sb3("T1", [128, 32, 33]); T2 = sb3("T2", [128, 32, 33]); T3 = sb3("T3", [128, 32, 33]); T4 = sb3("T4", [128, 32, 33])
            TI = sb3("TI", [128, 32, 33], I32)
            Gre = sb3("Gre", [128, 32, 8]); Gim = sb3("Gim", [128, 32, 8]); G8 = sb3("G8", [128, 32, 8])
            gre = sb3("gre", [128, 32]); gim = sb3("gim", [128, 32]); nre = sb3("nre", [128, 32]); den = sb3("den", [128, 32])
            Y16 = sb3("Y16", [128, 32, 16])

            def rr(x, xi, tmp):
                V(lambda e: e.tensor_copy(out=xi[:], in_=x[:]), [x], [xi])
                V(lambda e: e.tensor_copy(out=tmp[:], in_=xi[:]), [xi], [tmp])
                V(lambda e: e.tensor_tensor(out=x[:], in0=x[:], in1=tmp[:], op=ALU.subtract), [x, tmp], [x])
                V(lambda e: e.tensor_scalar(out=tmp[:], in0=x[:], scalar1=0.5, scalar2=None, op0=ALU.is_gt), [x], [tmp])
                V(lambda e: e.tensor_tensor(out=x[:], in0=x[:], in1=tmp[:], op=ALU.subtract), [x, tmp], [x])
                V(lambda e: e.tensor_scalar(out=tmp[:], in0=x[:], scalar1=-0.5, scalar2=None, op0=ALU.is_lt), [x], [tmp])
                V(lambda e: e.tensor_tensor(out=x[:], in0=x[:], in1=tmp[:], op=ALU.add), [x, tmp], [x])

            A(lambda e: e.activation(out=dt_t[:], in_=logdt[:], func=AF.Exp), [logdt], [dt_t])
            V(lambda e: e.tensor_tensor(out=a_t[:], in0=lamre[:], in1=dt_t[:], op=ALU.mult), [lamre, dt_t], [a_t])
            V(lambda e: e.scalar_tensor_tensor(out=u1[:], in0=lamim[:], scalar=1.0 / (2.0 * math.pi), in1=dt_t[:],
                                               op0=ALU.mult, op1=ALU.mult), [lamim, dt_t], [u1])
            rr(u1, i32, s32)
            kv_b = kvals.t[:, :].unsqueeze(1).broadcast_to([128, 32, 33])
            V(lambda e: e.tensor_tensor(out=T1[:], in0=bc_last(u1[:, :], 33), in1=kv_b, op=ALU.mult), [u1, kvals], [T1])
            rr(T1, TI, T2)
            A(lambda e: e.activation(out=T3[:], in_=T1[:], func=AF.Sin, scale=TWO_PI_S), [T1], [T3])
            V(lambda e: e.tensor_scalar(out=T1[:], in0=T1[:], scalar1=0.25, scalar2=None, op0=ALU.add), [T1], [T1])
            V(lambda e: e.tensor_scalar(out=T2[:], in0=T1[:], scalar1=0.5, scalar2=None, op0=ALU.is_gt), [T1], [T2])
            V(lambda e: e.tensor_tensor(out=T1[:], in0=T1[:], in1=T2[:], op=ALU.subtract), [T1, T2], [T1])
            A(lambda e: e.activation(out=T2[:], in_=T1[:], func=AF.Sin, scale=TWO_PI_S), [T1], [T2])
            V(lambda e: e.tensor_tensor(out=T4[:], in0=bc_last(a_t[:, :], 33), in1=kv_b, op=ALU.mult), [a_t, kvals], [T4])
            A(lambda e: e.activation(out=T4[:], in_=T4[:], func=AF.Exp), [T4], [T4])
            V(lambda e: e.tensor_tensor(out=T2[:], in0=T2[:], in1=T4[:], op=ALU.mult), [T2, T4], [T2])
            V(lambda e: e.tensor_tensor(out=T3[:], in0=T3[:], in1=T4[:], op=ALU.mult), [T3, T4], [T3])
            Lre, Lim = T2, T3
            V(lambda e: e.tensor_scalar(out=nre[:], in0=Lre[:, :, 16], scalar1=-1.0, scalar2=None, op0=ALU.add), [Lre], [nre])
            V(lambda e: e.tensor_tensor(out=den[:], in0=lamre[:], in1=lamre[:], op=ALU.mult), [lamre], [den])
            V(lambda e: e.tensor_tensor(out=s32[:], in0=lamim[:], in1=lamim[:], op=ALU.mult), [lamim], [s32])
            V(lambda e: e.tensor_tensor(out=den[:], in0=den[:], in1=s32[:], op=ALU.add), [den, s32], [den])
            V(lambda e: e.reciprocal(out=den[:], in_=den[:]), [den], [den])
            V(lambda e: e.tensor_tensor(out=gre[:], in0=nre[:], in1=lamre[:], op=ALU.mult), [nre, lamre], [gre])
            V(lambda e: e.tensor_tensor(out=s32[:], in0=Lim[:, :, 16], in1=lamim[:], op=ALU.mult), [Lim, lamim], [s32])
            V(lambda e: e.tensor_tensor(out=gre[:], in0=gre[:], in1=s32[:], op=ALU.add), [gre, s32], [gre])
            V(lambda e: e.tensor_tensor(out=gre[:], in0=gre[:], in1=den[:], op=ALU.mult), [gre, den], [gre])
            V(lambda e: e.tensor_tensor(out=gim[:], in0=Lim[:, :, 16], in1=lamre[:], op=ALU.mult), [Lim, lamre], [gim])
            V(lambda e: e.tensor_tensor(out=s32[:], in0=nre[:], in1=lamim[:], op=ALU.mult), [nre, lamim], [s32])
            V(lambda e: e.tensor_tensor(out=gim[:], in0=gim[:], in1=s32[:], op=ALU.subtract), [gim, s32], [gim])
            V(lambda e: e.tensor_tensor(out=gim[:], in0=gim[:], in1=den[:], op=ALU.mult), [gim, den], [gim])
            V(lambda e: e.tensor_tensor(out=Gre[:], in0=Lre[:, :, 0:8], in1=bc_last(gre[:, :], 8), op=ALU.mult), [Lre, gre], [Gre])
            V(lambda e: e.tensor_tensor(out=G8[:], in0=Lim[:, :, 0:8], in1=bc_last(gim[:, :], 8), op=ALU.mult), [Lim, gim], [G8])
            V(lambda e: e.tensor_tensor(out=Gre[:], in0=Gre[:], in1=G8[:], op=ALU.subtract), [Gre, G8], [Gre])
            V(lambda e: e.tensor_tensor(out=Gim[:], in0=Lre[:, :, 0:8], in1=bc_last(gim[:, :], 8), op=ALU.mult), [Lre, gim], [Gim])
            V(lambda e: e.tensor_tensor(out=G8[:], in0=Lim[:, :, 0:8], in1=bc_last(gre[:, :], 8), op=ALU.mult), [Lim, gre], [G8])
            V(lambda e: e.tensor_tensor(out=Gim[:], in0=Gim[:], in1=G8[:], op=ALU.add), [Gim, G8], [Gim])
            h0c, h1c, sgc, nh1c = halfc.t[:, 0:1], halfc.t[:, 1:2], halfc.t[:, 2:3], halfc.t[:, 3:4]

            def comb(out, p_, pc, q_, qc, op1, tmp):
                V(lambda e: e.tensor_scalar(out=tmp[:], in0=q_, scalar1=qc, scalar2=None, op0=ALU.mult), [Gre, Gim, T2, T3, halfc], [tmp])
                V(lambda e: e.scalar_tensor_tensor(out=out[:], in0=p_, scalar=pc, in1=tmp[:], op0=ALU.mult, op1=op1),
                  [Gre, Gim, T2, T3, halfc, tmp], [out])
            comb(X1, Gre[:], h0c, Gim[:], h1c, ALU.add, G8)
            comb(X2, Gre[:], h1c, Gim[:], h0c, ALU.subtract, G8)
            comb(Y1, Lre[:, :, 8:24], h0c, Lim[:, :, 8:24], h1c, ALU.subtract, Y16)
            comb(Y2, Lre[:, :, 8:24], nh1c, Lim[:, :, 8:24], h0c, ALU.subtract, Y16)
            V(lambda e: e.tensor_copy(out=crt[:], in_=Lre[:, :, 24:33]), [Lre], [crt])
            V(lambda e: e.tensor_scalar(out=cst[:], in0=Lim[:, :, 24:33], scalar1=sgc, scalar2=None, op0=ALU.mult), [Lim, halfc], [cst])
            V(lambda e: e.tensor_scalar(out=csn[:], in0=cst[:], scalar1=-1.0, scalar2=None, op0=ALU.mult), [cst], [csn])
            flush()
            es3.close()

            AT = sb2("AT", [128, 8, 128]); BM = sb2("BM", [128, 8, 128]); M2f = sb2("M2f", [128, 8, 128])
            tmp4 = sb2("tmp4", [128, 8, 128])
            M1b = sb2("M1b", [128, 8, 128], BF16)
            M2b = sb2("M2b", [128, 8, 128], BF16); Kmb = sb2("Kmb", [128, 8, 128], BF16)
            tmpK = sb2("tmpK", [128, 4, 128])
            U = sb2("U", [128, 8, 512], BF16)
            SA = sb2("SA", [128, 4, 512]); SB = sb2("SB", [128, 4, 512])
            Sp = [sb2("Sp%d" % i, [128, 4, 512], BF16) for i in range(2)]
            Yg = sb2("Yg", [128, 8, 512], BF16)
            for i in range(2):
                V(lambda e, i=i: e.memset(Sp[i][:, :, 0:1], 0.0), [], [Sp[i]])

            def v4(t):
                return t.t[:].rearrange("p g (j c) -> p g j c", j=8)

            def outer(dst, xa, ya, xb, yb, g0):
                def bx(x):
                    return x.t[:, g0:g0 + 8, :].unsqueeze(3).broadcast_to([128, 8, 8, 16])

                def by(y):
                    return y.t[:, g0:g0 + 8, :].unsqueeze(2).broadcast_to([128, 8, 8, 16])
                V(lambda e: e.tensor_tensor(out=v4(dst), in0=bx(xa), in1=by(ya), op=ALU.mult), [xa, ya], [dst])
                V(lambda e: e.tensor_tensor(out=v4(tmp4), in0=bx(xb), in1=by(yb), op=ALU.mult), [xb, yb], [tmp4])
                V(lambda e: e.tensor_tensor(out=dst[:], in0=dst[:], in1=tmp4[:], op=ALU.add), [dst, tmp4], [dst])

            Y1B = Tl(Y1.t[:, :, 0:8], Y1.tk); Y2B = Tl(Y2.t[:, :, 0:8], Y2.tk)
            Y1C = Tl(Y1.t[:, :, 8:16], Y1.tk); Y2C = Tl(Y2.t[:, :, 8:16], Y2.tk)
            ek = 0
            if SPARSE:
                wst = sb2("wst", [128, 4096], BF16)
            SAg = [Tl(SA.t[:, gl, :]) for gl in range(4)]
            SBg = [Tl(SB.t[:, gl, :]) for gl in range(4)]
            for gb in range(4):
                g0 = gb * 8
                if SPARSE:
                    for e16 in range(8):
                        ee = gb * 8 + e16
                        rows = slice(ee * 128, (ee + 1) * 128)
                        for a_, wd_ in enumerate((wg2_d, wu2_d)):
                            P.dma('pool', st_wst_in, lambda e, rows=rows, a_=a_, wd_=wd_: e.dma_start(
                                out=wst[:, a_ * 2048:(a_ + 1) * 2048], in_=wd_[rows, :]), writes=[wst])
                        P.dma('sp', st_wst_out, lambda e, rows=rows: e.dma_start(out=wbf_d[rows, 0:4096], in_=wst[:, 0:4096]), reads=[wst])
                        P.dma('pool', st_wst_in, lambda e, rows=rows: e.dma_start(out=wst[:, 0:2048], in_=wd2_d[rows, :]), writes=[wst])
                        P.dma('sp', st_wst_out, lambda e, rows=rows: e.dma_start(out=wbf_d[rows, 4096:6144], in_=wst[:, 0:2048]), reads=[wst])
                outer(AT, X1, bre, X2, bim, g0)
                outer(BM, Y1B, cre, Y2B, cim, g0)
                outer(M2f, Y1C, cre, Y2C, cim, g0)
                V(lambda e: e.tensor_copy(out=M2b[:], in_=M2f[:]), [M2f], [M2b])
                for hf in range(2):
                    pk = nextps()
                    for gl in range(4):
                        g = hf * 4 + gl
                        P.op('pe', lambda e, pk=pk, gl=gl, g=g: e.matmul(pk[:, gl * 128:(gl + 1) * 128], AT[:, g, :], BM[:, g, :],
                                                                        start=True, stop=True), reads=[AT, BM], writes=[pk])
                    V(lambda e, pk=pk: e.tensor_tensor(out=tmpK[:], in0=pk[:, :].rearrange("p (g m) -> p g m", g=4),
                                                       in1=maskK.t[:, :].unsqueeze(1).broadcast_to([128, 4, 128]), op=ALU.mult),
                      [pk, maskK], [tmpK])
                    for gl in range(4):
                        g = hf * 4 + gl
                        V(lambda e, gl=gl, g=g, g0=g0: e.scalar_tensor_tensor(
                            out=Kmb[:, g, :], in0=ident[:], scalar=dsk[:, g0 + g:g0 + g + 1], in1=tmpK[:, gl, :],
                            op0=ALU.mult, op1=ALU.add), [ident, dsk, tmpK], [Kmb])
                for src, dstb in ((AT, M1b),):
                    for hf in range(2):
                        pk = nextps()
                        for gl in range(4):
                            g = hf * 4 + gl
                            P.op('pe', lambda e, pk=pk, gl=gl, g=g, src=src: e.transpose(pk[:, gl * 128:(gl + 1) * 128], src[:, g, :], ident[:]),
                                 reads=[src, ident], writes=[pk])
                        evac(ek, dstb[:, hf * 4:(hf + 1) * 4, :], pk, dstb, pk[:, :].rearrange("p (g m) -> p g m", g=4))
                        ek += 1
                for g in range(8):
                    a_, par = g // 4, g % 4
                    pk = nextps()
                    for j in range(8):
                        P.op('pe', lambda e, pk=pk, a_=a_, par=par, j=j, gb=gb: e.matmul(
                            pk[:, :], selF[64 * a_:64 * a_ + 64, par, j, :], mixT[64 * a_:64 * a_ + 64, 4 + gb, j:S:8],
                            start=(j == 0), stop=(j == 7)), reads=[selF, mix_hi], writes=[pk])
                    evac(ek, U[:, g, :], pk, U)
                    ek += 1
                for un in range(2):
                    for gl in range(4):
                        g = un * 4 + gl
                        pk = nextps()
                        P.op('pe', lambda e, pk=pk, g=g: e.matmul(pk[:, :], M1b[:, g, :], U[:, g, :], start=True, stop=True),
                             reads=[M1b, U], writes=[pk])
                        evac(ek, SAg[gl][:, :], pk, SAg[gl])
                        ek += 1
                    spt = Sp[un]
                    srcg, dstg = SAg, SBg
                    for s_ in range(9):
                        d = 1 << s_
                        for gl in range(4):
                            G = g0 + un * 4 + gl
                            src0, dst0 = srcg[gl], dstg[gl]
                            A(lambda e, src0=src0, dst0=dst0, d=d: e.activation(out=dst0[:, 0:d], in_=src0[:, 0:d], func=AF.Copy),
                              [src0], [dst0])
                            pk = nextps()
                            P.op('pe', lambda e, pk=pk, src0=src0: e.matmul(pk[:, :], perm[:, :], src0[:, :], start=True, stop=True),
                                 reads=[perm, src0], writes=[pk])
                            V(lambda e, G=G, d=d, s_=s_, src0=src0, dst0=dst0: e.scalar_tensor_tensor(
                                out=dst0[:, d:512], in0=src0[:, 0:512 - d], scalar=crt[:, G, s_:s_ + 1],
                                in1=src0[:, d:512], op0=ALU.mult, op1=ALU.add), [src0, crt], [dst0])
                            V(lambda e, G=G, d=d, s_=s_, pk=pk, dst0=dst0: e.scalar_tensor_tensor(
                                out=dst0[:, d:512], in0=pk[:, 0:512 - d], scalar=cst[:, G, s_:s_ + 1],
                                in1=dst0[:, d:512], op0=ALU.mult, op1=ALU.add), [pk, cst, dst0], [dst0])
                        srcg, dstg = dstg, srcg
                    for gl in range(4):
                        Sfin = srcg[gl]
                        A(lambda e, Sfin=Sfin, spt=spt, gl=gl: e.activation(out=spt[:, gl, 1:512], in_=Sfin[:, 0:511], func=AF.Copy), [Sfin], [spt])
                    for gl in range(4):
                        g = un * 4 + gl
                        spt = Sp[un]
                        pk = nextps()
                        P.op('pe', lambda e, pk=pk, g=g: e.matmul(pk[:, :], Kmb[:, g, :], U[:, g, :], start=True, stop=False),
                             reads=[Kmb, U], writes=[pk])
                        P.op('pe', lambda e, pk=pk, g=g, gl=gl, spt=spt: e.matmul(pk[:, :], M2b[:, g, :], spt[:, gl, :], start=False, stop=True),
                             reads=[M2b, spt], writes=[pk])
                        A(lambda e, pk=pk, g=g: e.activation(out=Yg[:, g, :], in_=pk[:, :], func=AF.Gelu_apprx_tanh), [pk], [Yg])
                for i in range(8):
                    pk = nextps()
                    for g in range(8):
                        a_, par = g // 4, g % 4
                        P.op('pe', lambda e, pk=pk, a_=a_, par=par, i=i, g=g: e.matmul(
                            pk[64 * a_:64 * a_ + 64, :], selU[:, par, i, :], Yg[:, g, :], start=(par == 0), stop=(par == 3)),
                            reads=[selU, Yg], writes=[pk])
                    evac(ek, mixT[:, gb, i:S:8], pk, mix_lo)
                    ek += 1
            sgl = [sb2("sgl%d" % i, [128, 512], BF16) for i in range(2)]
            k = 0
            for mo in range(4):
                for tb in range(8):
                    tsl = slice(tb * 512, (tb + 1) * 512)
                    pk = nextps()
                    for ki in range(4):
                        P.op('pe', lambda e, pk=pk, ki=ki, mo=mo, tsl=tsl: e.matmul(
                            pk[:, :], wglu[:, ki, mo * 128:(mo + 1) * 128], mixT[:, ki, tsl], start=(ki == 0), stop=(ki == 3)),
                            reads=[wglu, mix_lo], writes=[pk])
                    sg_ = sgl[k % 2]
                    A(lambda e, pk=pk, sg_=sg_, mo=mo: e.activation(out=sg_[:], in_=pk[:, :], func=AF.Sigmoid, bias=bglu[:, mo:mo + 1], scale=1.0),
                      [pk, bglu], [sg_])
                    V(lambda e, sg_=sg_, mo=mo, tsl=tsl: e.tensor_tensor(out=mixT[:, 4 + mo, tsl], in0=sg_[:], in1=mixT[:, mo, tsl], op=ALU.mult),
                      [sg_, mix_lo], [mix_hi])
                    k += 1
            flush()
            es2.close()

        if do_att:
            es4 = ExitStack()

            def sb4(name, shape, dt=F32):
                return Tl(es4.enter_context(nc.sbuf_tensor(name, list(shape), dt)))
            xT = load_xT(es4)
            wq = sb4("wq", [128, 8, 64], BF16); wk = sb4("wk", [128, 8, 64], BF16); wvv = sb4("wvv", [128, 8, 128], BF16)
            st_w = [P.stream("wq"), P.stream("wk"), P.stream("wv")]
            qT = sb4("qT", [68, S], BF16); kT = sb4("kT", [68, S], BF16)
            st_qa = P.stream("qaug")
            vt = [sb4("vt%d" % b_, [128, 32, 2, 65], BF16) for b_ in range(3)]
            Oa = sb4("Oa", [65, S], F32)
            NPT = 3
            PT = [sb4("PT%d" % i, [128, 256], BF16) for i in range(NPT)]
            mask01 = sb4("mask01", [128, 256], BF16)
            P.dma('pool', P.stream("maskb"), lambda e: e.dma_start(out=mask01[:], in_=c_maskb_d[:, :]), writes=[mask01])
            P.dma('pool', P.stream("kaug"), lambda e: e.dma_start(out=kT[64:68, :], in_=c_kaug_d[:, :]), writes=[kT])
            lsel = sb4("lsel", [65, 128]); place = sb4("place", [65, 2, 128])
            P.dma('sp', P.stream("lsel"), lambda e: e.dma_start(out=lsel[:], in_=c_lsel_d[:, :]), writes=[lsel])
            P.dma('sp', P.stream("place"), lambda e: e.dma_start(out=place[:], in_=c_place_d[:, :, :]), writes=[place])
            Rt = sb4("Rt", [128, 512])
            if SPARSE:
                wst4 = sb4("wst4", [128, 2048], BF16)
            for b_ in range(3):
                V(lambda e, b_=b_: e.memset(vt[b_][:, :, :, 64:65], 1.0), [], [vt[b_]])
            units = []
            for n in range(32):
                units.append((0, 128 * n, 1, n - 1 if n > 0 else None, n))
            for r in range(4):
                for n in range(8):
                    units.append((1, 512 * n + r, 4, (r * 8 + n - 1) if n > 0 else None, r * 8 + n))
            for r in range(16):
                for n in range(2):
                    units.append((2, 2048 * n + r, 16, (r * 2 + n - 1) if n > 0 else None, r * 2 + n))

            def tok_ap(t, rows, base, dil):
                return t.t[rows, base:base + dil * 127 + 1:dil] if dil > 1 else t.t[rows, base:base + 128]
            PSS = [PS[0], PS[1]]; PSO = [PS[2], PS[3]]; PSP = [PS[4], PS[5]]; PSV = [PS[6], PS[7]]
            r68 = slice(0, 68)
            for h in range(8):
                pbase = (h % 2) * 64
                for i_, (wt_, off) in enumerate(((wq, 0), (wk, 512))):
                    P.dma('pool', st_w[i_], lambda e, wt_=wt_, off=off, h=h: e.dma_start(
                        out=wt_[:], in_=wv_in[:, :, off + h * 64:off + (h + 1) * 64]), writes=[wt_])
                if h % 2 == 0:
                    P.dma('pool', st_w[2], lambda e, h=h: e.dma_start(out=wvv[:], in_=wv_in[:, :, 1024 + h * 64:1024 + (h + 2) * 64]),
                          writes=[wvv])
                P.dma('pool', st_qa, lambda e, h=h: e.dma_start(out=qT[64:68, :], in_=c_qaug_d[h]), writes=[qT])
                if SPARSE:
                    for e4 in range(4):
                        ee = 32 + h * 4 + e4
                        rows = slice(ee * 128, (ee + 1) * 128)
                        for a_, wd_ in enumerate((wg2_d, wu2_d, wd2_d)):
                            P.dma('pool', st_wst_in, lambda e, rows=rows, wd_=wd_: e.dma_start(out=wst4[:, :], in_=wd_[rows, :]), writes=[wst4])
                            P.dma('sp', st_wst_out, lambda e, rows=rows, a_=a_: e.dma_start(out=wbf_d[rows, a_ * 2048:(a_ + 1) * 2048], in_=wst4[:, :]),
                                  reads=[wst4])
                k = 0
                for wt_, dst, scl in ((wq, qT, 0.125), (wk, kT, 1.0)):
                    for tb in range(8):
                        pk = PSP[k % 2]
                        for kc in range(8):
                            P.op('pe', lambda e, pk=pk, kc=kc, tb=tb, wt_=wt_: e.matmul(
                                pk[0:64, :], wt_[:, kc, :], xT[:, kc, tb * 512:(tb + 1) * 512], start=(kc == 0), stop=(kc == 7)),
                                reads=[wt_, xT], writes=[pk])
                        A(lambda e, pk=pk, dst=dst, tb=tb, scl=scl: e.activation(
                            out=dst[0:64, tb * 512:(tb + 1) * 512], in_=pk[0:64, :], func=AF.Copy, scale=scl), [pk], [dst])
                        k += 1
                k = 0
                for b_, dil in (((0, 1), (1, 4), (2, 16)) if h % 2 == 0 else ()):
                    for t4 in range(8):
                        pk = PSV[k % 2]
                        for tl in range(4):
                            tile_id = t4 * 4 + tl
                            if b_ == 0:
                                base = 128 * tile_id
                            elif b_ == 1:
                                base = 512 * (tile_id % 8) + tile_id // 8
                            else:
                                base = 2048 * (tile_id % 2) + tile_id // 2
                            for kc in range(8):
                                P.op('pe', lambda e, pk=pk, tl=tl, kc=kc, base=base, dil=dil: e.matmul(
                                    pk[:, tl * 128:(tl + 1) * 128],
                                    (xT[:, kc, base:base + dil * 127 + 1:dil] if dil > 1 else xT[:, kc, base:base + 128]),
                                    wvv[:, kc, :], start=(kc == 0), stop=(kc == 7)), reads=[xT, wvv], writes=[pk])
                        V(lambda e, pk=pk, b_=b_, t4=t4: e.tensor_copy(out=vt[b_][:, t4 * 4:(t4 + 1) * 4, :, 0:64],
                                                                       in_=pk[:, :].rearrange("p (t a e) -> p t a e", t=4, a=2)),
                          [pk], [vt[b_]])
                        k += 1

                def scores(ui):
                    b_, base, dil, tprev, tsame = units[ui]
                    pss = PSS[ui % 2]
                    qa = tok_ap(qT, r68, base, dil)
                    lo = 0 if tprev is not None else 128
                    P.op('pe', lambda e: e.matmul(pss[:, lo:256], identb[:, :], mask01[:, lo:256], start=True, stop=False),
                         reads=[identb, mask01], writes=[pss])
                    if tprev is not None:
                        kp = tok_ap(kT, r68, base - dil * 128, dil)
                        P.op('pe', lambda e: e.matmul(pss[:, 0:128], kp, qa, start=False, stop=False), reads=[kT, qT], writes=[pss])
                    ks = tok_ap(kT, r68, base, dil)
                    P.op('pe', lambda e: e.matmul(pss[:, 128:256], ks, qa, start=False, stop=True), reads=[kT, qT], writes=[pss])
                    pt = PT[ui % NPT]
                    A(lambda e: e.activation(out=pt[:, lo:256], in_=pss[:, lo:256], func=AF.Exp), [pss], [pt])

                def pv(ui):
                    b_, base, dil, tprev, tsame = units[ui]
                    hp = h % 2
                    pt = PT[ui % NPT]
                    pso = PSO[ui % 2]
                    if tprev is not None:
                        P.op('pe', lambda e: e.matmul(pso[0:65, 0:128], vt[b_][:, tprev, hp, :], pt[:, 0:128], start=True, stop=False),
                             reads=[vt[b_], pt], writes=[pso])
                    P.op('pe', lambda e: e.matmul(pso[0:65, 0:128], vt[b_][:, tsame, hp, :], pt[:, 128:256],
                                                  start=(tprev is None), stop=True), reads=[vt[b_], pt], writes=[pso])
                    oa = tok_ap(Oa, slice(0, 65), base, dil)
                    if b_ == 0:
                        V(lambda e: e.tensor_copy(out=oa, in_=pso[0:65, 0:128]), [pso], [Oa])
                    else:
                        V(lambda e: e.tensor_tensor(out=oa, in0=oa, in1=pso[0:65, 0:128], op=ALU.add), [pso, Oa], [Oa])
                nu = len(units)
                for ui in range(nu):
                    scores(ui)
                    if ui > 0:
                        pv(ui - 1)
                pv(nu - 1)
                for tb in range(8):
                    tsl = slice(tb * 512, (tb + 1) * 512)
                    pl_, po_ = PSP[0], PSP[1]
                    P.op('pe', lambda e, tsl=tsl, pl_=pl_: e.matmul(pl_[:, :], lsel[:, :], Oa[0:65, tsl], start=True, stop=True),
                         reads=[lsel, Oa], writes=[pl_])
                    P.op('pe', lambda e, tsl=tsl, po_=po_, h=h: e.matmul(po_[:, :], place[:, h % 2, :], Oa[0:65, tsl], start=True, stop=True),
                         reads=[place, Oa], writes=[po_])
                    V(lambda e, pl_=pl_: e.reciprocal(out=Rt[:], in_=pl_[:, :]), [pl_], [Rt])
                    V(lambda e, po_=po_, tsl=tsl, h=h, pbase=pbase: e.tensor_tensor(
                        out=mixT[pbase:pbase + 64, h // 2, tsl], in0=po_[pbase:pbase + 64, :], in1=Rt[pbase:pbase + 64, :], op=ALU.mult),
                        [po_, Rt], [mix_lo])
            flush()
            es4.close()

        if debug.get("mix_only"):
            st_md = P.stream("mixout")
            mo_v = mixout_d.rearrange("(c p) t -> p c t", p=128)
            for c in range(0 if do_att else 4, 8 if do_ssm else 4):
                P.dma('pool', st_md, lambda e, c=c: e.dma_start(out=mo_v[:, c, :], in_=mixT[:, c, :]), reads=[mixT, mix_lo, mix_hi])
            P.ops['pool'].append(([(st_md, P.cnt[st_md])], None, None))
            flush()
            es_mix.close()
            es.close()
            return nc

    esP = ExitStack()
    if SPARSE:
        wt_all = Tl(esP.enter_context(nc.sbuf_tensor("wt_all", [128, 32, 64], F32)))
        rank_all = Tl(esP.enter_context(nc.sbuf_tensor("rank_all", [128, 32, 64], F32)))
    esB = ExitStack()
    esA = ExitStack()
    sb_main = sb

    def sb(name, shape, dt=F32):
        return Tl(esA.enter_context(nc.sbuf_tensor(name, list(shape), dt)))

    acc = sb("acc", [128, 8, QT], F32); hT = sb("hT", [128, 8, QT], BF16)
    pTb = sb("pTb", [128, 2, QT], BF16); wtT = sb("wtT", [64, QT], BF16)
    acc_v = acc.t[:]; hT_v = hT.t[:]; pT_v = pTb.t[:]; wtT_v = wtT.t[:]

    slot = [sb("wslot%d" % i, [128, 6144], BF16) for i in range(2)] if not SPARSE else [None, None]
    st_slot = [P.stream("slot%d" % i) for i in range(2)]
    xt = [sb("xt%d" % i, [128, D], F32) for i in range(2)]
    st_x = [P.stream("x%d" % i) for i in range(2)]
    rt = [sb("rt%d" % i, [128, D], F32) for i in range(2)]
    lng = sb("lng", [128, D], F32); lnb = sb("lnb", [128, D], F32)
    st_ln = P.stream("ln"); st_ln2 = P.stream("ln2")
    wple = sb("wple", [128, 2, D], BF16)
    st_wple = P.stream("wple")
    wr = sb("wr", [128, 8, 64], F32)
    rbias = sb("rbias", [128, 64], F32)
    st_p = P.stream("pT")
    st_out = [P.stream("out%d" % i) for i in range(2)]
    sgt = [sb("sgt%d" % i, [128, 512], BF16) for i in range(2)]
    t1t = [sb("t1t%d" % i, [128, 512], BF16) for i in range(2)]
    actw = [[sb("actw%d_%d" % (i, j), [128, 512], BF16) for j in range(2)] for i in range(2)]
    oh = [sb("oh%d" % i, [64, 128], BF16) for i in range(2)] if not SPARSE else [None, None]
    sig = [sb("sig%d" % i, [128, 512], F32) for i in range(2)]
    stats = sb("stats", [128, 2, 6], F32)
    mv_t = sb("mv_t", [128, 2], F32)
    rstd = sb("rstd", [128, 1], F32)
    nb_t = sb("nb_t", [128, 1], F32)
    sc = sb("sc", [128, 64], F32); sel = sb("sel", [128, 64], F32); eq = sb("eq", [128, 64], F32)
    sel2 = sb("sel2", [128, 64], F32); m1 = sb("m1", [128, 8], F32); m2 = sb("m2", [128, 8], F32)
    gs = sb("gs", [128, 8], F32); top8 = sb("top8", [128, 8], F32); pen = sb("pen", [128, 8], F32)
    selm = sb("selm", [128, 64], F32); msk = sb("msk", [128, 64], F32); wsel = sb("wsel", [128, 64], F32)
    ssum = sb("ssum", [128, 1], F32); wt = sb("wt", [128, 64], F32)

    if not (debug.get('skipdma', 0) & 1):
      P.dma('sp', P.stream("wr"), lambda e: e.dma_start(out=wr[:], in_=wr_d.rearrange("(c p) e -> p c e", p=128)), writes=[wr])
    if not (debug.get('skipdma', 0) & 2):
      P.dma('sp', P.stream("rbias"), lambda e: e.dma_start(out=rbias[:], in_=rb_d.partition_broadcast(128)), writes=[rbias])
    if not (debug.get('skipdma', 0) & 4):
      P.dma('pool', st_wple, lambda e: e.dma_start(out=wple[:], in_=wple_d.rearrange("(c p) d -> p c d", p=128)),
          writes=[wple])

    def load_ln(g_d, b_d):
        P.dma('sp', st_ln, lambda e: e.dma_start(out=lng[:], in_=g_d.partition_broadcast(128)), writes=[lng])
        P.dma('sp', st_ln2, lambda e: e.dma_start(out=lnb[:], in_=b_d.partition_broadcast(128)), writes=[lnb])

    LN_POOL = [bool(debug.get('ln_pool', True))]

    def layer_norm(src, dst):
        for h in range(2):
            P.op('dve', lambda e, h=h: e.bn_stats(out=stats[:, h, :], in_=src[:, h * 512:(h + 1) * 512]),
                 reads=[src], writes=[stats])
        P.op('dve', lambda e: e.bn_aggr(out=mv_t[:], in_=stats[:].rearrange("p a b -> p (a b)")),
             reads=[stats], writes=[mv_t])
        P.op('act', lambda e: e.activation(out=rstd[:], in_=mv_t[:, 1:2], func=AF.Sqrt, bias=EPS, scale=1.0),
             reads=[mv_t], writes=[rstd])
        P.op('dve', lambda e: e.reciprocal(out=rstd[:], in_=rstd[:]), reads=[rstd], writes=[rstd])
        P.op('dve', lambda e: e.scalar_tensor_tensor(out=nb_t[:], in0=mv_t[:, 0:1], scalar=-1.0, in1=rstd[:],
                                                     op0=ALU.mult, op1=ALU.mult),
             reads=[mv_t, rstd], writes=[nb_t])
        P.op('dve', lambda e: e.tensor_scalar(out=dst[:], in0=src[:], scalar1=rstd[:, 0:1], scalar2=nb_t[:, 0:1],
                                              op0=ALU.mult, op1=ALU.add),
             reads=[src, nb_t, rstd], writes=[dst])
        eng_ = 'pool' if LN_POOL[0] else 'dve'
        P.op(eng_, lambda e: e.tensor_tensor(out=dst[:], in0=dst[:], in1=lng[:], op=ALU.mult),
             reads=[dst, lng], writes=[dst])
        P.op(eng_, lambda e: e.tensor_tensor(out=dst[:], in0=dst[:], in1=lnb[:], op=ALU.add),
             reads=[dst, lnb], writes=[dst])

    def load_big(w_d):
        wv = w_d.rearrange("(c p) d -> p c d", p=128)
        for h in range(2):
            sv = slot[h].t[:, 0:4096].rearrange("p (c d) -> p c d", c=8)
            P.dma('pool', st_slot[h], lambda e, h=h, sv=sv: e.dma_start(out=sv, in_=wv[:, :, h * 512:(h + 1) * 512]),
                  writes=[slot[h]])

    RES = SPARSE
    if RES:
        wout_t = sb("wout_t", [128, 8, D], BF16); wpg_t = sb("wpg_t", [128, 8, D], BF16); wsh_t = sb("wsh_t", [128, 6144], BF16)
        P.dma('pool', P.stream("wout_t"), lambda e: e.dma_start(out=wout_t[:], in_=w_out_d.rearrange("(c p) d -> p c d", p=128)), writes=[wout_t])
        P.dma('pool', P.stream("wpg_t"), lambda e: e.dma_start(out=wpg_t[:], in_=wpg_d.rearrange("(c p) d -> p c d", p=128)), writes=[wpg_t])
        st_wsh = P.stream("wsh_t")
        P.dma('pool', st_wsh, lambda e: e.dma_start(out=wsh_t.t[:, 0:2048].rearrange("p (c h) -> p c h", c=8),
                                                   in_=wsg_d.rearrange("(c p) h -> p c h", p=128)), writes=[wsh_t])
        P.dma('pool', st_wsh, lambda e: e.dma_start(out=wsh_t.t[:, 2048:4096].rearrange("p (c h) -> p c h", c=8),
                                                   in_=wsu_d.rearrange("(c p) h -> p c h", p=128)), writes=[wsh_t])
        P.dma('pool', st_wsh, lambda e: e.dma_start(out=wsh_t.t[:, 4096:6144].rearrange("p (c d) -> p c d", c=2),
                                                   in_=wsd_d.rearrange("(c p) d -> p c d", p=128)), writes=[wsh_t])

    def slot_big(h):
        return slot[h].t[:, 0:4096].rearrange("p (c d) -> p c d", c=8)

    def load_expert(e_idx, s):
        if e_idx < 64:
            g_d, u_d, d_d = wg_d[e_idx], wu_d[e_idx], wd_d[e_idx]
        else:
            g_d, u_d, d_d = wsg_d, wsu_d, wsd_d
        sg_v = slot[s].t[:, 0:2048].rearrange("p (c h) -> p c h", c=8)
        su_v = slot[s].t[:, 2048:4096].rearrange("p (c h) -> p c h", c=8)
        sd_v = slot[s].t[:, 4096:6144].rearrange("p (c d) -> p c d", c=2)
        P.dma('pool', st_slot[s], lambda e: e.dma_start(out=sg_v, in_=g_d.rearrange("(c p) h -> p c h", p=128)),
              writes=[slot[s]])
        P.dma('pool', st_slot[s], lambda e: e.dma_start(out=su_v, in_=u_d.rearrange("(c p) h -> p c h", p=128)),
              writes=[slot[s]])
        P.dma('pool', st_slot[s], lambda e: e.dma_start(out=sd_v, in_=d_d.rearrange("(c p) d -> p c d", p=128)),
              writes=[slot[s]])
        return sg_v, su_v, sd_v

    def sparse_moe():
        nonlocal sb, slot, xt, rt, lng, lnb, stats, mv_t, rstd, nb_t
        flush()
        esA.close()

        def sb(name, shape, dt=F32):
            return Tl(esB.enter_context(nc.sbuf_tensor(name, list(shape), dt)))
        slot = [sb("wslotB%d" % i, [128, 6144], BF16) for i in range(2)]
        xtb_ = sb("xtB0", [128, D], F32)
        xt = [xtb_, xtb_]
        rt = [sb("rtB%d" % i, [128, D], F32) for i in range(2)]
        lng = sb("lngB", [128, D], F32); lnb = sb("lnbB", [128, D], F32)
        stats = sb("statsB", [128, 2, 6], F32); mv_t = sb("mv_tB", [128, 2], F32)
        rstd = sb("rstdB", [128, 1], F32); nb_t = sb("nb_tB", [128, 1], F32)
        tokid = sb("tokid", [128, 32])
        zrow = sb("zrow", [128, 8], BF16)
        V(lambda e: e.memset(zrow[:], 0.0), [], [zrow])
        P.dma('sp', P.stream("zrow"), lambda e: e.dma_start(out=Hs_d[S:S + 1, :].rearrange("a (p c) -> (a p) c", p=128), in_=zrow[:]), reads=[zrow])
        pre = sb("pre", [128, 1280], F32)
        P.dma('sp', P.stream("pre"), lambda e: e.dma_start(out=pre[:], in_=c_prefill_d[:, :]), writes=[pre])
        btw_t = Tl(None)
        P.dma('sp', P.stream("prefill"), lambda e: e.dma_start(out=btw_d.rearrange("(p r) c -> p (r c)", p=128), in_=pre[:]),
              reads=[pre], writes=[btw_t]); pcol = sb("pcol", [128, 1]); bvals = sb("bvals", [128, 320])
        P.dma('sp', P.stream("tokid"), lambda e: e.dma_start(out=tokid[:], in_=c_tokid_d[:, :]), writes=[tokid])
        P.dma('sp', P.stream("pcol"), lambda e: e.dma_start(out=pcol[:], in_=c_pcol_d[:, :]), writes=[pcol])
        P.dma('sp', P.stream("bvals"), lambda e: e.dma_start(out=bvals[:], in_=c_bvals_d[:, :]), writes=[bvals])
        pc = sb("pc", [128, 64]); pci = sb("pci", [128, 64], I32); t64 = sb("t64", [128, 64]); t64b = sb("t64b", [128, 64])
        pend = sb("pend", [128, 64]); pstart = sb("pstart", [128, 64]); ones64 = sb("ones64", [128, 64])
        V(lambda e: e.memset(ones64[:], 1.0), [], [ones64])
        V(lambda e: e.tensor_scalar(out=t64[:], in0=cum[:], scalar1=127.0, scalar2=1.0 / 128.0, op0=ALU.add, op1=ALU.mult), [cum], [t64])
        V(lambda e: e.tensor_copy(out=pci[:], in_=t64[:]), [t64], [pci])
        V(lambda e: e.tensor_copy(out=pc[:], in_=pci[:]), [pci], [pc])
        V(lambda e: e.tensor_scalar(out=t64[:], in0=cum[:], scalar1=127.0, scalar2=None, op0=ALU.add), [cum], [t64])
        V(lambda e: e.tensor_scalar(out=t64b[:], in0=pc[:], scalar1=128.0, scalar2=None, op0=ALU.mult), [pc], [t64b])
        V(lambda e: e.tensor_tensor(out=t64b[:], in0=t64b[:], in1=t64[:], op=ALU.is_gt), [t64b, t64], [t64b])
        V(lambda e: e.tensor_tensor(out=pc[:], in0=pc[:], in1=t64b[:], op=ALU.subtract), [pc, t64b], [pc])
        V(lambda e: e.tensor_scalar(out=pc[:], in0=pc[:], scalar1=128.0, scalar2=None, op0=ALU.mult), [pc], [pc])
        V(lambda e: e.tensor_tensor_scan(out=pend[:], data0=ones64[:], data1=pc[:], initial=0.0, op0=ALU.mult, op1=ALU.add),
          [ones64, pc], [pend])
        V(lambda e: e.tensor_tensor(out=pstart[:], in0=pend[:], in1=pc[:], op=ALU.subtract), [pend, pc], [pstart])
        key = sb("key", [128, 64]); junk = sb("junk", [128, 64])
        d8 = sb("d8", [128, 32, 8]); d8i = sb("d8i", [128, 32, 8], I32); pay = sb("pay", [128, 32, 8, 4])
        V(lambda e: e.memset(pay[:], 0.0), [], [pay])
        for T_ in range(32):
            V(lambda e, T_=T_: e.tensor_tensor(out=key[:], in0=rank_all[:, T_, :], in1=pstart[:], op=ALU.add), [rank_all, pstart], [key])
            V(lambda e, T_=T_: e.tensor_scalar(out=junk[:], in0=wt_all[:, T_, :], scalar1=0.0, scalar2=None, op0=ALU.is_gt), [wt_all], [junk])
            V(lambda e, T_=T_: e.scalar_tensor_tensor(out=key[:], in0=key[:], scalar=1.0, in1=junk[:], op0=ALU.add, op1=ALU.mult),
              [key, junk], [key])
            V(lambda e: e.tensor_scalar(out=key[:], in0=key[:], scalar1=-1.0, scalar2=None, op0=ALU.add), [key], [key])
            V(lambda e, T_=T_: e.max(out=d8[:, T_, :], in_=key[:]), [key], [d8])
            for k in range(8):
                V(lambda e, T_=T_, k=k: e.scalar_tensor_tensor(out=junk[:], in0=key[:], scalar=d8[:, T_, k:k + 1], in1=wt_all[:, T_, :],
                                                              op0=ALU.is_equal, op1=ALU.mult, accum_out=pay[:, T_, k, 1:2]),
                  [key, d8, wt_all], [junk, pay])
            V(lambda e, T_=T_: e.tensor_copy(out=pay[:, T_, :, 0], in_=tokid[:, T_:T_ + 1].broadcast_to([128, 8])), [tokid], [pay])
        V(lambda e: e.tensor_copy(out=d8i[:], in_=d8[:]), [d8], [d8i])
        st_sc = P.stream("scatter")
        for T_ in range(32):
            for k in range(8):
                P.dma('pool', st_sc, lambda e, T_=T_, k=k: e.indirect_dma_start(
                    out=btw_d[:, :], out_offset=IOA(ap=d8i[:, T_, k:k + 1], axis=0), in_=pay[:, T_, k, :], in_offset=None,
                    bounds_check=breg(e, 40959), oob_is_err=False), reads=[d8i, pay, btw_t])
        blk = sb("blk", [128, 320]); cmpb = sb("cmpb", [128, 16, 64]); idxw = sb("idxw", [128, 320]); idxwi = sb("idxwi", [128, 320], I32)
        same = sb("same", [128, 320])
        for ch in range(20):
            V(lambda e, ch=ch: e.tensor_tensor(out=cmpb[:], in0=pend.t[:, :].unsqueeze(1).broadcast_to([128, 16, 64]),
                                               in1=bc_last(bvals[:, ch * 16:(ch + 1) * 16], 64), op=ALU.is_le), [pend, bvals], [cmpb])
            V(lambda e, ch=ch: e.tensor_reduce(out=blk[:, ch * 16:(ch + 1) * 16], in_=cmpb[:], axis=AX.X, op=ALU.add), [cmpb], [blk])
        V(lambda e: e.tensor_scalar(out=blk[:], in0=blk[:], scalar1=63.0, scalar2=None, op0=ALU.min), [blk], [blk])
        V(lambda e: e.tensor_scalar(out=idxw[:], in0=blk[:], scalar1=128.0, scalar2=pcol[:, 0:1], op0=ALU.mult, op1=ALU.add), [blk, pcol], [idxw])
        NSL_ = debug.get('nsl', 4)
        if debug.get("wskip", False):
            V(lambda e: e.tensor_tensor(out=same[:, NSL_:320], in0=blk[:, NSL_:320], in1=blk[:, 0:320 - NSL_], op=ALU.is_equal), [blk], [same])
            V(lambda e: e.scalar_tensor_tensor(out=idxw[:, NSL_:320], in0=same[:, NSL_:320], scalar=1.0e6, in1=idxw[:, NSL_:320],
                                               op0=ALU.mult, op1=ALU.add), [same, idxw], [idxw])
        V(lambda e: e.tensor_copy(out=idxwi[:], in_=idxw[:]), [idxw], [idxwi])
        P.barrier()
        NSL = debug.get('nsl', 4)
        slot3 = [slot[0], slot[1]] + [sb("wslotC%d" % i, [128, 6144], BF16) for i in range(NSL - 2)]
        st_slot3 = [st_slot[0], st_slot[1]] + [P.stream("slotC%d" % i) for i in range(NSL - 2)]
        tw_all = sb("tw_all", [128, 320, 4]); twi_all = sb("twi_all", [128, 320], I32)
        P.dma('sp', P.stream("tw_all"), lambda e: e.dma_start(out=tw_all[:], in_=btw_d.rearrange("(b p) c -> p b c", p=128)), writes=[tw_all])
        V(lambda e: e.tensor_copy(out=twi_all[:], in_=tw_all[:, :, 0]), [tw_all], [twi_all])
        NXG = 4
        xg = [sb("xg%d" % i, [128, D], BF16) for i in range(NXG)]; st_xg = [P.stream("xg%d" % i) for i in range(NXG)]
        xgT = [sb("xgT%d" % i, [128, 8, 128], BF16) for i in range(3)]
        sgb = [sb("sgb%d" % i, [128, 256], BF16) for i in range(2)]; actb = [sb("actb%d" % i, [128, 256], BF16) for i in range(2)]
        yr = [sb("yr%d" % i, [128, D], BF16) for i in range(2)]; st_yr = [P.stream("yr%d" % i) for i in range(2)]
        NB = debug.get("nblk", 320)
        PTs = [[PS[0], PS[1]], [PS[6], PS[7]]]
        PGU = [PS[2], PS[3]]
        PD = [PS[4], PS[5]]

        def wviews(b):
            sl_ = slot3[b % NSL]
            return (sl_.t[:, 0:2048].rearrange("p (c h) -> p c h", c=8), sl_.t[:, 2048:4096].rearrange("p (c h) -> p c h", c=8),
                    sl_.t[:, 4096:6144].rearrange("p (c d) -> p c d", c=2), sl_)

        def S0a(b):
            x3 = b % NXG
            P.dma('pool', st_xg[x3], lambda e: e.indirect_dma_start(
                out=xg[x3][:, :], out_offset=None, in_=Hs_d[:, :], in_offset=IOA(ap=twi_all[:, b:b + 1], axis=0),
                bounds_check=breg(e, S), oob_is_err=False), reads=[twi_all], writes=[xg[x3]])

        def S0b(b):
            w3 = b % NSL
            P.dma('pool', st_slot3[w3], lambda e: e.indirect_dma_start(
                out=slot3[w3].t[:, :], out_offset=None, in_=wbf_d[:, :],
                in_offset=IOA(ap=idxwi[:, b:b + 1], axis=0), bounds_check=breg(e, 8191), oob_is_err=False),
                reads=[idxwi], writes=[slot3[w3]])

        def S1(b):
            x3 = b % 3
            xq = b % NXG
            pt0, pt1 = PTs[b % 2]
            for dc in range(8):
                pk = pt0 if dc < 4 else pt1
                P.op('pe', lambda e, pk=pk, dc=dc: e.matmul(pk[:, (dc % 4) * 128:(dc % 4 + 1) * 128], xg[xq][:, dc * 128:(dc + 1) * 128],
                                                          identb[:, :], start=True, stop=True), reads=[xg[xq], identb], writes=[pk])
            A(lambda e: e.activation(out=xgT[x3][:, 0:4, :], in_=pt0[:, :].rearrange("p (c r) -> p c r", c=4), func=AF.Copy),
              [pt0], [xgT[x3]])
            V(lambda e: e.tensor_copy(out=xgT[x3][:, 4:8, :], in_=pt1[:, :].rearrange("p (c r) -> p c r", c=4)), [pt1], [xgT[x3]])

        def S2(b):
            x3, a2 = b % 3, b % 2
            sg_v, su_v, sd_v, sl_ = wviews(b)
            pg, pu = PGU
            for pk, wv_ in ((pg, sg_v), (pu, su_v)):
                for hb in range(2):
                    for kc in range(8):
                        P.op('pe', lambda e, pk=pk, wv_=wv_, hb=hb, kc=kc: e.matmul(
                            pk[:, hb * 128:(hb + 1) * 128], wv_[:, kc, hb * 128:(hb + 1) * 128], xgT[x3][:, kc, :],
                            start=(kc == 0), stop=(kc == 7)), reads=[sl_, xgT[x3]], writes=[pk])
            A(lambda e: e.activation(out=sgb[a2][:], in_=pg[:, 0:256], func=AF.Silu), [pg], [sgb[a2]])
            V(lambda e: e.tensor_tensor(out=actb[a2][:], in0=sgb[a2][:], in1=pu[:, 0:256], op=ALU.mult), [sgb[a2], pu], [actb[a2]])

        def S3(b):
            a2 = b % 2
            sg_v, su_v, sd_v, sl_ = wviews(b)
            pd0, pd1 = PD
            for half, pk in ((0, pd0), (1, pd1)):
                for hb in range(2):
                    P.op('pe', lambda e, pk=pk, half=half, hb=hb: e.matmul(
                        pk[:, :], actb[a2][:, hb * 128:(hb + 1) * 128], sd_v[:, hb, half * 512:(half + 1) * 512],
                        start=(hb == 0), stop=(hb == 1)), reads=[actb[a2], sl_], writes=[pk])
            A(lambda e: e.activation(out=yr[a2][:, 0:512], in_=pd0[:, :], func=AF.Copy, scale=tw_all[:, b, 1:2]), [pd0, tw_all], [yr[a2]])
            V(lambda e: e.tensor_scalar(out=yr[a2][:, 512:1024], in0=pd1[:, :], scalar1=tw_all[:, b, 1:2], scalar2=None, op0=ALU.mult),
              [pd1, tw_all], [yr[a2]])
            P.dma('sp', st_yr[a2], lambda e: e.dma_start(out=yrows_d[b * 128:(b + 1) * 128, :], in_=yr[a2][:]), reads=[yr[a2]])

        for b in range(min(NXG, NB)):
            S0a(b)
        for b in range(min(NSL, NB)):
            S0b(b)
        for i in range(-2, NB):
            if 0 <= i + 2 < NB:
                S1(i + 2)
            if 0 <= i + 1 < NB:
                S2(i + 1)
            if 0 <= i:
                S3(i)
            if NXG <= i + 2 + NXG < NB:
                S0a(i + 2 + NXG)
            if NSL <= i + NSL < NB:
                S0b(i + NSL)
        P.barrier()
        LN_POOL[0] = False
        load_ln(ln2g_d, ln2b_d)
        NYG = 3
        yg = [sb("yg%d" % i, [128, D], BF16) for i in range(NYG)]; st_yg = [P.stream("yg%d" % i) for i in range(NYG)]
        for T_ in range(32):
            b2 = T_ % 2
            P.dma('sp', st_x[b2], lambda e, b2=b2, T_=T_: e.dma_start(out=rt[b2][:], in_=Zs_d[T_ * 128:(T_ + 1) * 128, :]), writes=[rt[b2]])
            pa0, pa1 = PS[2 * b2], PS[2 * b2 + 1]
            for k in range(8):
                j = (T_ * 8 + k) % NYG
                P.dma('pool', st_yg[j], lambda e, j=j, T_=T_, k=k: e.indirect_dma_start(
                    out=yg[j][:, :], out_offset=None, in_=yrows_d[:, :], in_offset=IOA(ap=d8i[:, T_, k:k + 1], axis=0),
                    bounds_check=breg(e, 40959), oob_is_err=False), reads=[d8i], writes=[yg[j]])
                for hf, pk in ((0, pa0), (1, pa1)):
                    P.op('pe', lambda e, j=j, hf=hf, pk=pk, k=k: e.matmul(pk[:, :], identb[:, :], yg[j][:, hf * 512:(hf + 1) * 512],
                                                                         start=(k == 0), stop=(k == 7)), reads=[identb, yg[j]], writes=[pk])
            for hf, pk in ((0, pa0), (1, pa1)):
                V(lambda e, b2=b2, hf=hf, pk=pk: e.tensor_tensor(out=rt[b2][:, hf * 512:(hf + 1) * 512], in0=rt[b2][:, hf * 512:(hf + 1) * 512],
                                                              in1=pk[:, :], op=ALU.add), [rt[b2], pk], [rt[b2]])
            layer_norm(rt[b2], xt[b2])
            P.dma('sp', st_out[b2], lambda e, b2=b2, T_=T_: e.dma_start(out=out_d[T_ * 128:(T_ + 1) * 128, :], in_=xt[b2][:]), reads=[xt[b2]])

    stages = debug.get('stages', 'F1F2F3F4F5')
    def F1(q, acc, hT, pTb, acc_v, hT_v, pT_v):
        t0 = q * QT
        if not RES:
            load_big(w_out_d)
        if not (debug.get('skipdma', 0) & 8):
            load_ln(ln1g_d, ln1b_d)
        pv = pT_d.rearrange("(c p) t -> p c t", p=128)
        if not (debug.get('skipdma', 0) & 16):
          P.dma('pool', st_p, lambda e, t0=t0: e.dma_start(out=pT_v, in_=pv[:, :, t0:t0 + QT]), writes=[pTb])
        for tt in range(NTT):
            tok = t0 + tt * 128
            b = tt % 2
            P.dma('sp', st_x[b], lambda e, b=b, tok=tok: e.dma_start(out=xt[b][:], in_=x_d[tok:tok + 128, :]),
                  writes=[xt[b]])
            for h in range(2):
                for kc in range(8):
                    P.op('pe', lambda e, h=h, kc=kc, tok=tok: e.matmul(
                        PS[h][:, :], mixT[:, kc, tok:tok + 128],
                        (wout_t[:, kc, h * 512:(h + 1) * 512] if RES else slot_big(h)[:, kc, :]),
                        start=(kc == 0), stop=(kc == 7)), reads=[mixT, (wout_t if RES else slot[h])], writes=[PS[h]])
                P.op('dve', lambda e, h=h, b=b: e.scalar_tensor_tensor(
                    out=rt[b][:, h * 512:(h + 1) * 512], in0=xt[b][:, h * 512:(h + 1) * 512], scalar=ALPHA,
                    in1=PS[h][:, :], op0=ALU.mult, op1=ALU.add), reads=[xt[b], PS[h]], writes=[rt[b]])
            lvl = debug.get('f1lvl', 3)
            if lvl >= 2:
                layer_norm(rt[b], xt[b])
            if SPARSE:
                P.dma('pool', st_hs[b], lambda e, b=b, tok=tok: e.dma_start(out=Hs_d[tok:tok + 128, :], in_=xt[b][:]), reads=[xt[b]])
            if lvl == 1:
                P.dma('sp', st_out[b], lambda e, b=b, tok=tok: e.dma_start(out=out_d[tok:tok + 128, :], in_=rt[b][:]), reads=[rt[b]])
            if lvl == 2:
                P.dma('sp', st_out[b], lambda e, b=b, tok=tok: e.dma_start(out=out_d[tok:tok + 128, :], in_=xt[b][:]), reads=[xt[b]])
            if tt > 0:
                f1_tail(tt - 1, acc, hT, acc_v, hT_v)
        f1_tail(NTT - 1, acc, hT, acc_v, hT_v)

    def f1_tail(tt, acc, hT, acc_v, hT_v):
        b = tt % 2
        for g4 in range(2):
            pb = PS[2 + g4]
            for j in range(4):
                dc = g4 * 4 + j
                P.op('pe', lambda e, pb=pb, j=j, dc=dc, b=b: e.transpose(
                    pb[:, j * 128:(j + 1) * 128], xt[b][:, dc * 128:(dc + 1) * 128], ident[:]),
                    reads=[xt[b], ident], writes=[pb])
            P.op('act', lambda e, pb=pb, g4=g4, tt=tt: e.activation(
                out=hT_v[:, g4 * 4:(g4 + 1) * 4, tt * 128:(tt + 1) * 128],
                in_=pb[:, :].rearrange("p (c t) -> p c t", c=4), func=AF.Copy), reads=[pb], writes=[hT, pb])
            P.op('dve', lambda e, pb=pb, g4=g4, tt=tt: e.tensor_scalar(
                out=acc_v[:, g4 * 4:(g4 + 1) * 4, tt * 128:(tt + 1) * 128],
                in0=pb[:, :].rearrange("p (c t) -> p c t", c=4), scalar1=ALPHA, scalar2=None,
                op0=ALU.mult), reads=[pb], writes=[acc])

    def F2(q, acc, hT, pTb, acc_v, hT_v, pT_v):
        t0 = q * QT
        for tt in range(NTT if 'F2' in stages else 0):
            pl = PS[4 + tt % 2]
            for kc in range(8):
                P.op('pe', lambda e, pl=pl, kc=kc, tt=tt: e.matmul(
                    pl[:, 0:64], acc_v[:, kc, tt * 128:(tt + 1) * 128], wr[:, kc, :],
                    start=(kc == 0), stop=(kc == 7)), reads=[acc, wr], writes=[pl])
            P.op('act', lambda e, pl=pl: e.activation(out=sc[:], in_=pl[:, 0:64], func=AF.Sigmoid, scale=1.0 / ALPHA),
                 reads=[pl], writes=[sc])
            P.op('dve', lambda e: e.tensor_tensor(out=sel[:], in0=sc[:], in1=rbias[:], op=ALU.add),
                 reads=[sc, rbias], writes=[sel])
            sel3 = sel.t[:, :].rearrange("p (g k) -> p g k", g=8)
            P.op('dve', lambda e: e.tensor_reduce(out=m1[:], in_=sel3, axis=AX.X, op=ALU.max),
                 reads=[sel], writes=[m1])
            P.op('dve', lambda e: e.tensor_tensor(out=eq.t[:, :].rearrange("p (g k) -> p g k", g=8), in0=sel3,
                                                  in1=bc_last(m1[:, :], 8), op=ALU.is_equal),
                 reads=[sel, m1], writes=[eq])
            P.op('dve', lambda e: e.scalar_tensor_tensor(out=sel2[:], in0=eq[:], scalar=-1e9, in1=sel[:],
                                                         op0=ALU.mult, op1=ALU.add),
                 reads=[eq, sel], writes=[sel2])
            P.op('dve', lambda e: e.tensor_reduce(out=m2[:], in_=sel2.t[:, :].rearrange("p (g k) -> p g k", g=8),
                                                  axis=AX.X, op=ALU.max), reads=[sel2], writes=[m2])
            P.op('dve', lambda e: e.tensor_tensor(out=gs[:], in0=m1[:], in1=m2[:], op=ALU.add),
                 reads=[m1, m2], writes=[gs])
            P.op('dve', lambda e: e.max(out=top8[:], in_=gs[:]), reads=[gs], writes=[top8])
            P.op('dve', lambda e: e.tensor_scalar(out=pen[:], in0=gs[:], scalar1=top8[:, 3:4], scalar2=1e9,
                                                  op0=ALU.is_ge, op1=ALU.mult), reads=[gs, top8], writes=[pen])
            P.op('dve', lambda e: e.tensor_scalar(out=pen[:], in0=pen[:], scalar1=-1e9, scalar2=None, op0=ALU.add),
                 reads=[pen], writes=[pen])
            P.op('dve', lambda e: e.tensor_tensor(out=selm.t[:, :].rearrange("p (g k) -> p g k", g=8), in0=sel3,
                                                  in1=bc_last(pen[:, :], 8), op=ALU.add),
                 reads=[sel, pen], writes=[selm])
            P.op('dve', lambda e: e.max(out=top8[:], in_=selm[:]), reads=[selm], writes=[top8])
            P.op('dve', lambda e: e.tensor_scalar(out=msk[:], in0=selm[:], scalar1=top8[:, 7:8], scalar2=None,
                                                  op0=ALU.is_ge), reads=[selm, top8], writes=[msk])
            P.op('dve', lambda e: e.tensor_tensor(out=wsel[:], in0=sc[:], in1=msk[:], op=ALU.mult),
                 reads=[sc, msk], writes=[wsel])
            P.op('dve', lambda e: e.tensor_reduce(out=ssum[:], in_=wsel[:], axis=AX.X, op=ALU.add),
                 reads=[wsel], writes=[ssum])
            P.op('dve', lambda e: e.reciprocal(out=ssum[:], in_=ssum[:]), reads=[ssum], writes=[ssum])
            P.op('dve', lambda e: e.tensor_scalar(out=wt[:], in0=wsel[:], scalar1=ssum[:, 0:1], scalar2=2.5,
                                                  op0=ALU.mult, op1=ALU.mult), reads=[wsel, ssum], writes=[wt])
            if SPARSE:
                T_ = q * NTT + tt
                P.op('dve', lambda e, T_=T_: e.tensor_copy(out=mskb[:, T_ % NTT, :], in_=msk[:]), reads=[msk], writes=[mskb])
                P.op('dve', lambda e, T_=T_: e.tensor_copy(out=wt_all[:, T_, :], in_=wt[:]), reads=[wt], writes=[wt_all])
            else:
                pw = PS[6]
                P.op('pe', lambda e, pw=pw: e.transpose(pw[0:64, 0:128], wt[:, :], ident[:]),
                     reads=[wt, ident], writes=[pw])
                P.op('act', lambda e, pw=pw, tt=tt: e.activation(out=wtT_v[0:64, tt * 128:(tt + 1) * 128],
                                                                 in_=pw[0:64, 0:128], func=AF.Copy),
                     reads=[pw], writes=[wtT])
    def F2b(q):
        for tt in range(NTT if ('F2' in stages and SPARSE) else 0):
            T_ = q * NTT + tt
            pw = PS[6 + tt % 2]
            P.op('pe', lambda e, pw=pw, tt=tt: e.matmul(pw[:, 0:64], trib[:, :], mskb[:, tt, :], start=True, stop=True),
                 reads=[trib, mskb], writes=[pw])
            P.op('pe', lambda e, pw=pw, tt=tt: e.matmul(pw[:, 64:128], onesb[:, :], mskb[:, tt, :], start=True, stop=True),
                 reads=[onesb, mskb], writes=[pw])
            P.op('dve', lambda e, pw=pw, T_=T_: e.tensor_tensor(out=rank_all[:, T_, :], in0=pw[:, 0:64], in1=cum[:], op=ALU.add),
                 reads=[pw, cum], writes=[rank_all])
            P.op('dve', lambda e, pw=pw: e.tensor_tensor(out=cum[:], in0=pw[:, 64:128], in1=cum[:], op=ALU.add),
                 reads=[pw, cum], writes=[cum])

    def F345(q, acc, hT, pTb, acc_v, hT_v, pT_v):
        t0 = q * QT
        NE = debug.get('ne', 65) if 'F3' in stages else 0
        E0 = 64 if SPARSE else 0
        pending = (load_expert(E0, E0 % 2) if NE else None) if not RES else None
        for ei in range(E0, NE):
            s = ei % 2
            if RES:
                sg_v = wsh_t.t[:, 0:2048].rearrange("p (c h) -> p c h", c=8)
                su_v = wsh_t.t[:, 2048:4096].rearrange("p (c h) -> p c h", c=8)
                sd_v = wsh_t.t[:, 4096:6144].rearrange("p (c d) -> p c d", c=2)
                wtile = wsh_t
            else:
                sg_v, su_v, sd_v = pending
                wtile = slot[s]
                if ei + 1 < NE:
                    pending = load_expert(ei + 1, (ei + 1) % 2)
            if ei < 64:
                P.op('act', lambda e, s=s, ei=ei: e.activation(
                    out=oh[s][:], in_=identb[0:64, ei:ei + 1].broadcast_to([64, 128]), func=AF.Copy),
                    reads=[identb], writes=[oh[s]])
            for tb in range(NTB):
                tsl = slice(tb * 512, (tb + 1) * 512)
                if ei < 64:
                    P.op('pe', lambda e, s=s, tsl=tsl: e.matmul(PS[4][:, :], oh[s][:, :], wtT_v[0:64, tsl],
                                                                start=True, stop=True),
                         reads=[oh[s], wtT], writes=[PS[4]])
                for hb in range(2):
                    pg, pu = PS[hb], PS[2 + hb]
                    for kc in range(8):
                        P.op('pe', lambda e, pg=pg, kc=kc, hb=hb, tsl=tsl, sg_v=sg_v: e.matmul(
                            pg[:, :], sg_v[:, kc, hb * 128:(hb + 1) * 128], hT_v[:, kc, tsl],
                            start=(kc == 0), stop=(kc == 7)), reads=[wtile, hT], writes=[pg])
                    for kc in range(8):
                        P.op('pe', lambda e, pu=pu, kc=kc, hb=hb, tsl=tsl, su_v=su_v: e.matmul(
                            pu[:, :], su_v[:, kc, hb * 128:(hb + 1) * 128], hT_v[:, kc, tsl],
                            start=(kc == 0), stop=(kc == 7)), reads=[wtile, hT], writes=[pu])
                    P.op('act', lambda e, pg=pg, hb=hb: e.activation(out=sgt[hb][:], in_=pg[:, :], func=AF.Silu),
                         reads=[pg], writes=[sgt[hb]])
                    if ei < 64:
                        P.op('dve', lambda e, pu=pu, hb=hb: e.tensor_tensor(out=t1t[hb][:], in0=sgt[hb][:],
                                                                            in1=pu[:, :], op=ALU.mult),
                             reads=[sgt[hb], pu], writes=[t1t[hb]])
                        P.op('dve', lambda e, hb=hb, tb=tb: e.tensor_tensor(out=actw[tb][hb][:], in0=t1t[hb][:],
                                                                            in1=PS[4][:, :], op=ALU.mult),
                             reads=[t1t[hb], PS[4]], writes=[actw[tb][hb]])
                    else:
                        P.op('dve', lambda e, pu=pu, hb=hb, tb=tb: e.tensor_tensor(
                            out=actw[tb][hb][:], in0=sgt[hb][:], in1=pu[:, :], op=ALU.mult),
                            reads=[sgt[hb], pu], writes=[actw[tb][hb]])
                for dc in range(8):
                    po = PS[5 + dc % 3]
                    for hb in range(2):
                        P.op('pe', lambda e, po=po, hb=hb, dc=dc, tb=tb, sd_v=sd_v: e.matmul(
                            po[:, :], sd_v[:, hb, dc * 128:(dc + 1) * 128], actw[tb][hb][:, :],
                            start=(hb == 0), stop=(hb == 1)), reads=[wtile, actw[tb][hb]], writes=[po])
                    P.op('dve', lambda e, po=po, dc=dc, tsl=tsl: e.tensor_tensor(
                        out=acc_v[:, dc, tsl], in0=acc_v[:, dc, tsl], in1=po[:, :], op=ALU.add),
                        reads=[acc, po], writes=[acc])
        if 'F4' in stages:
            if not RES:
                load_big(wpg_d)
        for tb in range(NTB if 'F4' in stages else 0):
            tsl = slice(tb * 512, (tb + 1) * 512)
            for dc in range(8):
                pa, pbk = PS[dc % 2], PS[2 + dc % 2]
                for kc in range(2):
                    P.op('pe', lambda e, pa=pa, kc=kc, dc=dc, tsl=tsl: e.matmul(
                        pa[:, :], wple[:, kc, dc * 128:(dc + 1) * 128], pT_v[:, kc, tsl],
                        start=(kc == 0), stop=(kc == 1)), reads=[wple, pTb], writes=[pa])
                hh = dc // 4
                for kc in range(8):
                    P.op('pe', lambda e, pbk=pbk, kc=kc, dc=dc, tsl=tsl, hh=hh: e.matmul(
                        pbk[:, :], (wpg_t[:, kc, dc * 128:(dc + 1) * 128] if RES else slot_big(hh)[:, kc, (dc % 4) * 128:(dc % 4 + 1) * 128]),
                        hT_v[:, kc, tsl],
                        start=(kc == 0), stop=(kc == 7)), reads=[(wpg_t if RES else slot[hh]), hT], writes=[pbk])
                sg_ = sig[dc % 2]
                P.op('act', lambda e, pbk=pbk, sg_=sg_: e.activation(out=sg_[:], in_=pbk[:, :], func=AF.Sigmoid),
                     reads=[pbk], writes=[sg_])
                P.op('dve', lambda e, pa=pa, sg_=sg_: e.tensor_tensor(out=sg_[:], in0=sg_[:], in1=pa[:, :], op=ALU.mult),
                     reads=[sg_, pa], writes=[sg_])
                P.op('dve', lambda e, sg_=sg_, dc=dc, tsl=tsl: e.tensor_tensor(
                    out=acc_v[:, dc, tsl], in0=acc_v[:, dc, tsl], in1=sg_[:], op=ALU.add),
                    reads=[acc, sg_], writes=[acc])
        if 'F5' in stages and not SPARSE:
            load_ln(ln2g_d, ln2b_d)
        for tt in range(NTT if 'F5' in stages else 0):
            b = tt % 2
            tok = t0 + tt * 128
            for g4 in range(2):
                pb = PS[2 * b + g4]
                for j in range(4):
                    dc = g4 * 4 + j
                    P.op('pe', lambda e, pb=pb, j=j, dc=dc, tt=tt: e.transpose(
                        pb[:, j * 128:(j + 1) * 128], acc_v[:, dc, tt * 128:(tt + 1) * 128], ident[:]),
                        reads=[acc, ident], writes=[pb])
                P.op('act', lambda e, pb=pb, g4=g4, b=b: e.activation(out=rt[b][:, g4 * 512:(g4 + 1) * 512],
                                                                     in_=pb[:, :], func=AF.Copy),
                     reads=[pb], writes=[rt[b]])
            if SPARSE:
                P.dma('sp', st_zs[b], lambda e, b=b, tok=tok: e.dma_start(out=Zs_d[tok:tok + 128, :], in_=rt[b][:]), reads=[rt[b]])
            else:
                layer_norm(rt[b], xt[b])
                P.dma('sp', st_out[b], lambda e, b=b, tok=tok: e.dma_start(out=out_d[tok:tok + 128, :], in_=xt[b][:]),
                      reads=[xt[b]])

    bufs = [(acc, hT, pTb, acc_v, hT_v, pT_v)]
    nq_ = debug.get('nq', NQ)
    for q in range(nq_):
        F1(q, *bufs[0])
        F2(q, *bufs[0])
        F345(q, *bufs[0])
        F2b(q)
    P.barrier()
    if SPARSE:
        sparse_moe()

    P.ops['sp'].append(([(st_out[0], P.cnt[st_out[0]]), (st_out[1], P.cnt[st_out[1]])], None, None))
    flush()
    esB.close()
    esA.close()
    esP.close()
    es_mix.close()
    es.close()
    return nc


def make_consts():
    c = {}
    c["c_ident"] = np.eye(128, dtype=np.float32)
    selF = np.zeros((128, 4, 8, 128), np.float32)
    for a in range(2):
        for q in range(4):
            for j in range(8):
                for cin in range(16):
                    selF[64 * a + q * 16 + cin, q, j, j * 16 + cin] = 1.0
    c["c_selF2"] = selF.reshape(128, 4096)
    selU = np.zeros((128, 4, 8, 64), np.float32)
    for i in range(8):
        for q in range(4):
            for co in range(16):
                selU[i * 16 + co, q, i, q * 16 + co] = 1.0
    c["c_selU2"] = selU.reshape(128, 2048)
    jj = np.arange(128) // 16
    c["c_maskK"] = (jj[None, :] >= jj[:, None]).astype(np.float32)
    kv = [7 - j for j in range(8)] + [i - 7 for i in range(8)] + [i + 1 for i in range(8)] + [8 * (1 << s) for s in range(9)]
    c["c_kvals"] = np.tile(np.asarray(kv, np.float32)[None, :], (128, 1))
    half = (np.arange(128) >= 64).astype(np.float32)
    c["c_half"] = np.stack([1.0 - half, half, 2.0 * half - 1.0, -half], axis=1).astype(np.float32)
    ki = np.arange(128)[:, None]; qi = np.arange(128)[None, :]
    NEG = -30000.0
    c["c_maskb"] = np.concatenate([np.where(ki >= qi, 0.0, NEG), np.where(ki <= qi, 0.0, NEG)], axis=1).astype(np.float32)
    t = np.arange(S)
    a_t = (t // 64).astype(np.float32); b_t = (t % 64).astype(np.float32)
    slopes = np.asarray([2.0 ** (-(h + 1)) for h in range(8)], np.float32)
    qaug = np.zeros((8, 4, S), np.float32)
    for h in range(8):
        qaug[h, 0] = -slopes[h] * 64.0 * a_t
        qaug[h, 1] = -slopes[h] * b_t
        qaug[h, 2] = slopes[h]
        qaug[h, 3] = slopes[h]
    c["c_qaug"] = qaug
    c["c_kaug"] = np.stack([np.ones(S, np.float32), np.ones(S, np.float32), 64.0 * a_t, b_t], axis=0).astype(np.float32)
    lsel = np.zeros((65, 128), np.float32); lsel[64, :] = 1.0
    c["c_lsel"] = lsel
    place = np.zeros((65, 2, 128), np.float32)
    for e in range(64):
        place[e, 0, e] = 1.0
        place[e, 1, 64 + e] = 1.0
    c["c_place"] = place
    c["c_perm"] = np.roll(np.eye(128, dtype=np.float32), 64, axis=1)
    c["c_tri"] = (np.arange(128)[:, None] < np.arange(128)[None, :]).astype(np.float32)
    c["c_tokid"] = (np.arange(32)[None, :] * 128 + np.arange(128)[:, None]).astype(np.float32)
    c["c_pcol"] = np.arange(128, dtype=np.float32)[:, None].copy()
    c["c_bvals"] = np.tile((np.arange(320, dtype=np.float32) * 128.0)[None, :], (128, 1))
    c["c_prefill"] = np.tile(np.asarray([float(S), 0.0, 0.0, 0.0], np.float32), (128, 320))
    return c


def core_inputs(inputs, b, consts):
    g = lambda k: np.asarray(inputs[k])[0]
    m = dict(consts)
    x = np.asarray(inputs["x"])
    m["x"] = np.ascontiguousarray(x[b])
    m["xT"] = np.ascontiguousarray(x[b].T)
    m["pT"] = np.ascontiguousarray(g("p")[b].T)
    for k in ("w_in", "w_out", "ln1_g", "ln1_b", "ln2_g", "ln2_b", "w_router", "router_bias",
              "ws_gate", "ws_up", "ws_down", "w_ple", "w_ple_gate", "w_glu"):
        m[k] = np.ascontiguousarray(g(k))
    if "wg2" not in _WCACHE:
        _WCACHE["wg2"] = np.ascontiguousarray(g("w_gate").reshape(64, 8, 128, 256).transpose(0, 2, 1, 3).reshape(8192, 2048))
        _WCACHE["wu2"] = np.ascontiguousarray(g("w_up").reshape(64, 8, 128, 256).transpose(0, 2, 1, 3).reshape(8192, 2048))
        _WCACHE["wd2"] = np.ascontiguousarray(g("w_down").reshape(64, 2, 128, 1024).transpose(0, 2, 1, 3).reshape(8192, 2048))
    m["wg2"] = _WCACHE["wg2"]; m["wu2"] = _WCACHE["wu2"]; m["wd2"] = _WCACHE["wd2"]
    m["lamre2"] = np.ascontiguousarray(np.tile(g("lam_re").T, (2, 1)))
    m["lamim2"] = np.ascontiguousarray(np.tile(g("lam_im").T, (2, 1)))
    m["logdt2"] = np.ascontiguousarray(np.tile(g("log_dt")[None, :], (128, 1)))
    m["bre2"] = np.ascontiguousarray(np.tile(g("b_re").transpose(1, 0, 2), (2, 1, 1)))
    m["bim2"] = np.ascontiguousarray(np.tile(g("b_im").transpose(1, 0, 2), (2, 1, 1)))
    m["cre2"] = np.ascontiguousarray(np.tile(g("c_re").transpose(2, 0, 1), (2, 1, 1)))
    m["cim2"] = np.ascontiguousarray(np.tile(g("c_im").transpose(2, 0, 1), (2, 1, 1)))
    m["dsk2"] = np.ascontiguousarray(np.tile(g("d_skip").T, (8, 1)))
    m["bglu2"] = np.ascontiguousarray(g("b_glu").reshape(4, 128).T)
    return m


_NC = [None]
_WCACHE = {}


def kernel(**inputs):
    if _NC[0] is None:
        _NC[0] = build()
    nc = _NC[0]
    consts = make_consts()
    _WCACHE.clear()
    in_maps = [core_inputs(inputs, b, consts) for b in range(8)]
    res = run_bass_kernel_spmd(nc, in_maps, core_ids=list(range(8)))
    return np.stack([r["out"] for r in res.results], axis=0).astype(np.float32)
```
